# Optimizing a Trainium2 kernel written in Bass

```python
import math
import jax, jax.numpy as jnp
from jax import lax
import numpy as np

D_MODEL = 1024
BATCH = 8
SEQ = 8192
DEPTH = 1

HEAD_DIM = 64
NA_HEADS = 8
NA_WIDTH = NA_HEADS * HEAD_DIM
NA_KR_MAX = 8
NA_KC = 16
GRID_W = 64
WG_HEADS = 8
WG_KV_HEADS = 2
WG_WIDTH = WG_HEADS * HEAD_DIM
WG_KV_WIDTH = WG_KV_HEADS * HEAD_DIM
WINDOW = 128
WG_BLOCK = 128
N_EXPERTS = 16
EC_CAPACITY_FACTOR = 2
D_FF = 2048
IN_SPLITS = (NA_WIDTH, NA_WIDTH, NA_WIDTH, WG_WIDTH, WG_KV_WIDTH, WG_KV_WIDTH, D_MODEL, D_MODEL)
IN_WIDTH = sum(IN_SPLITS)
LN_EPS = 1e-5
DN_ALPHA = (2 * DEPTH) ** 0.25
DN_BETA = (8 * DEPTH) ** -0.25
NEG_INF = -1e30

kernel_name = "hybrid_natten_wgqa_ecmoe_deepnorm"


def layer_norm(x, g, b):
    xf = x.astype(jnp.float32)
    mu = jnp.mean(xf, -1, keepdims=True)
    var = jnp.mean(jnp.square(xf - mu), -1, keepdims=True)
    return ((xf - mu) * lax.rsqrt(var + LN_EPS) * g + b).astype(x.dtype)


def alibi_slopes(n_heads):
    return jnp.exp2(-8.0 * (jnp.arange(n_heads, dtype=jnp.float32) + 1.0) / n_heads)


def neighbourhood_attention(q, k, v, rpb):
    B, S, H, Dh = q.shape
    rows = S // GRID_W
    kr = min(NA_KR_MAX, rows)
    qg = (q * (Dh ** -0.5)).reshape(B, rows, GRID_W, H, Dh)
    kg = k.reshape(B, rows, GRID_W, H, Dh)
    vg = v.reshape(B, rows, GRID_W, H, Dh)
    cols = jnp.arange(GRID_W)
    col_start = jnp.clip(cols - NA_KC // 2, 0, GRID_W - NA_KC)
    col_idx = col_start[:, None] + jnp.arange(NA_KC)[None, :]
    col_off = col_idx - cols[:, None] + (NA_KC - 1)

    def row_block(r):
        r0 = jnp.clip(r - kr // 2, 0, rows - kr)
        q_r = lax.dynamic_index_in_dim(qg, r, axis=1, keepdims=False)
        k_r = lax.dynamic_slice_in_dim(kg, r0, kr, axis=1)
        v_r = lax.dynamic_slice_in_dim(vg, r0, kr, axis=1)
        k_win = k_r[:, :, col_idx]
        v_win = v_r[:, :, col_idx]
        row_off = r0 + jnp.arange(kr) - r + (NA_KR_MAX - 1)
        bias = rpb[:, row_off[:, None, None], col_off[None, :, :]]
        s = jnp.einsum('bchd,bicjhd->bhcij', q_r, k_win).astype(jnp.float32)
        s = s + bias.transpose(0, 2, 1, 3)[None].astype(jnp.float32)
        p = jax.nn.softmax(s.reshape(B, H, GRID_W, kr * NA_KC), axis=-1)
        p = p.reshape(B, H, GRID_W, kr, NA_KC).astype(v.dtype)
        return jnp.einsum('bhcij,bicjhd->bchd', p, v_win)

    out = lax.map(row_block, jnp.arange(rows))
    return out.transpose(1, 0, 2, 3, 4).reshape(B, S, H * Dh)


def windowed_gqa_sample(q, k, v, sink):
    S, HQ, Dh = q.shape
    HKV = k.shape[1]
    G = HQ // HKV
    L = WG_BLOCK
    nb = S // L
    qb = (q * (Dh ** -0.5)).reshape(nb, L, HKV, G, Dh)

    def band(t):
        tb = jnp.pad(t.reshape(nb, L, HKV, Dh), ((1, 1), (0, 0), (0, 0), (0, 0)))
        return jnp.concatenate([tb[:-2], tb[1:-1], tb[2:]], axis=1)

    kb, vb = band(k), band(v)
    s = jnp.einsum('nqkgd,nskd->nkgqs', qb, kb).astype(jnp.float32)
    q_loc = jnp.arange(L)[:, None]
    s_loc = jnp.arange(3 * L)[None, :] - L
    dist = jnp.abs(s_loc - q_loc).astype(jnp.float32)
    key_pos = jnp.arange(nb)[:, None] * L + s_loc
    valid = (dist <= WINDOW)[None] & ((key_pos >= 0) & (key_pos < S))[:, None, :]
    slopes = alibi_slopes(HQ).reshape(HKV, G)
    s = s - slopes[None, :, :, None, None] * dist[None, None, None]
    s = jnp.where(valid[:, None, None], s, NEG_INF)
    sink_l = sink.astype(jnp.float32).reshape(HKV, G)[None, :, :, None, None]
    m = jnp.maximum(jnp.max(s, -1, keepdims=True), sink_l)
    e = jnp.exp(s - m)
    p = e / (jnp.sum(e, -1, keepdims=True) + jnp.exp(sink_l - m))
    o = jnp.einsum('nkgqs,nskd->nqkgd', p.astype(v.dtype), vb)
    return o.reshape(S, HQ * Dh)


def expert_choice_moe(x, w_router, w_gate, w_up, w_down):
    B, n, D = x.shape
    E = w_router.shape[1]
    cap = EC_CAPACITY_FACTOR * n // E
    aff = jax.nn.softmax(jnp.einsum('bnd,de->ben', x, w_router).astype(jnp.float32), axis=1)
    gates, idx = lax.top_k(aff, cap)
    idx_e = idx.transpose(1, 0, 2)
    xin = x[jnp.arange(B)[None, :, None], idx_e]

    def expert(args):
        xe, wg, wu, wd = args
        h = jax.nn.silu(xe @ wg) * (xe @ wu)
        return h @ wd

    out = lax.map(expert, (xin, w_gate, w_up, w_down))
    out = out * gates.transpose(1, 0, 2)[..., None].astype(out.dtype)

    def combine(ib, ob):
        return jnp.zeros((n, D), ob.dtype).at[ib.reshape(-1)].add(ob.reshape(-1, D))

    return jax.vmap(combine, in_axes=(1, 1))(idx_e, out)


def setup_inputs(seed: int = 0) -> dict:
    key = jax.random.key(seed)
    ks = jax.random.split(key, 17)
    f = jnp.float32
    nrm = lambda k, shape, s: jax.random.normal(k, shape, f) * s
    L = DEPTH
    return {
        "x": nrm(ks[0], (BATCH, SEQ, D_MODEL), 1.0),
        "w_in": nrm(ks[1], (L, D_MODEL, IN_WIDTH), D_MODEL ** -0.5),
        "b_in": nrm(ks[2], (L, IN_WIDTH), 0.01),
        "rpb": nrm(ks[3], (L, NA_HEADS, 2 * NA_KR_MAX - 1, 2 * NA_KC - 1), 0.02),
        "sink": nrm(ks[4], (L, WG_HEADS), 0.5),
        "w_branch_a": nrm(ks[5], (L, NA_WIDTH, D_MODEL), DN_BETA * NA_WIDTH ** -0.5),
        "w_branch_b": nrm(ks[6], (L, WG_WIDTH, D_MODEL), DN_BETA * WG_WIDTH ** -0.5),
        "w_out": nrm(ks[7], (L, D_MODEL, D_MODEL), DN_BETA * D_MODEL ** -0.5),
        "ln1_g": 1.0 + nrm(ks[8], (L, D_MODEL), 0.02),
        "ln1_b": nrm(ks[9], (L, D_MODEL), 0.01),
        "w_router": nrm(ks[10], (L, D_MODEL, N_EXPERTS), D_MODEL ** -0.5),
        "w_gate": nrm(ks[11], (L, N_EXPERTS, D_MODEL, D_FF), D_MODEL ** -0.5),
        "w_up": nrm(ks[12], (L, N_EXPERTS, D_MODEL, D_FF), D_MODEL ** -0.5),
        "w_down": nrm(ks[13], (L, N_EXPERTS, D_FF, D_MODEL), DN_BETA * D_FF ** -0.5),
        "ln2_g": 1.0 + nrm(ks[14], (L, D_MODEL), 0.02),
        "ln2_b": nrm(ks[15], (L, D_MODEL), 0.01),
    }


def reference(x, w_in, b_in, rpb, sink, w_branch_a, w_branch_b, w_out, ln1_g, ln1_b,
              w_router, w_gate, w_up, w_down, ln2_g, ln2_b):
    B, S, D = x.shape
    offsets = list(np.cumsum(IN_SPLITS)[:-1])
    for l in range(DEPTH):
        proj = jnp.einsum('bsd,de->bse', x, w_in[l]) + b_in[l]
        qa, ka, va, qb, kb, vb, ga, gb = jnp.split(proj, offsets, axis=-1)
        ya = neighbourhood_attention(qa.reshape(B, S, NA_HEADS, HEAD_DIM),
                                     ka.reshape(B, S, NA_HEADS, HEAD_DIM),
                                     va.reshape(B, S, NA_HEADS, HEAD_DIM), rpb[l])
        sink_l = sink[l]
        yb = lax.map(lambda a: windowed_gqa_sample(a[0], a[1], a[2], sink_l),
                     (qb.reshape(B, S, WG_HEADS, HEAD_DIM),
                      kb.reshape(B, S, WG_KV_HEADS, HEAD_DIM),
                      vb.reshape(B, S, WG_KV_HEADS, HEAD_DIM)))
        mix = (jax.nn.sigmoid(ga) * (ya @ w_branch_a[l])
               + jax.nn.sigmoid(gb) * (yb @ w_branch_b[l]))
        x = layer_norm(DN_ALPHA * x + mix @ w_out[l], ln1_g[l], ln1_b[l])
        moe = expert_choice_moe(x, w_router[l], w_gate[l], w_up[l], w_down[l])
        x = layer_norm(DN_ALPHA * x + moe, ln2_g[l], ln2_b[l])
    return x
```

```python
import numpy as np
import concourse.bass as bass
import concourse.mybir as mybir
from concourse.bass_utils import run_bass_kernel_spmd

F32 = mybir.dt.float32
BF16 = mybir.dt.bfloat16
I32 = mybir.dt.int32
AF = mybir.ActivationFunctionType
ALU = mybir.AluOpType
AX = mybir.AxisListType

EPOCH = 30000


class Buf:
    __slots__ = ("name", "w", "r", "dsem", "dcnt", "dcls")

    def __init__(self, name):
        self.name = name
        self.w = {}
        self.r = {}
        self.dsem = None
        self.dcnt = 0
        self.dcls = None


class Eng:
    def __init__(self, prog, name, eng):
        self.prog, self.name, self.eng = prog, name, eng
        self.n = 0
        self.sems = []
        self.seen = {}
        self.own = set()

    def next_event(self):
        ep, off = divmod(self.n, EPOCH)
        while len(self.sems) <= ep:
            s = self.prog.new_sem(f"{self.name}_e{len(self.sems)}")
            self.sems.append(s)
            self.own.add(id(s))
        self.n += 1
        return (self.sems[ep], off + 1)


class Prog:
    def __init__(self, nc):
        self.nc = nc
        self._keep = []
        self._sems = []
        self.free_sems = {}
        self.nsem = 0
        self.pe = Eng(self, "pe", nc.tensor)
        self.act = Eng(self, "act", nc.scalar)
        self.dve = Eng(self, "dve", nc.vector)
        self.pool = Eng(self, "pool", nc.gpsimd)
        self.sp = Eng(self, "sp", nc.sync)
        self.engs = [self.pe, self.act, self.dve, self.pool, self.sp]

    def new_sem(self, name):
        cm = self.nc.semaphore(name)
        s = cm.__enter__()
        self._sems.append(cm)
        self.nsem += 1
        return s

    def sb(self, name, shape, dt):
        cm = self.nc.sbuf_tensor("s_" + name, list(shape), dt)
        t = cm.__enter__()
        self._keep.append(cm)
        return t

    def ps(self, name, shape, dt):
        cm = self.nc.psum_tensor("p_" + name, list(shape), dt)
        t = cm.__enter__()
        self._keep.append(cm)
        return t

    def _deps(self, E, reads, writes):
        need = {}

        def add(ev, raw):
            sem, c = ev
            if id(sem) in E.own and not raw:
                return
            k = id(sem)
            if k not in need or need[k][1] < c:
                need[k] = (sem, c)

        for b in reads:
            for ev in b.w.values():
                add(ev, True)
        for b in writes:
            for ev in b.w.values():
                add(ev, False)
            for ev in b.r.values():
                add(ev, False)
        for k, (sem, c) in need.items():
            if E.seen.get(k, 0) < c:
                E.eng.wait_ge(sem, c)
                E.seen[k] = c

    def _commit(self, ev, reads, writes):
        k = id(ev[0])
        for b in writes:
            b.w = {k: ev}
            b.r = {}
        for b in reads:
            b.r[k] = ev

    def op(self, E, fn, reads=(), writes=()):
        self._deps(E, reads, writes)
        ins = fn(E.eng)
        ev = E.next_event()
        ins.then_inc(ev[0], 1)
        self._commit(ev, reads, writes)
        return ins

    def dma(self, E, fn, semb, reads=(), writes=(), adds=()):
        self._deps(E, reads, writes)
        if semb.dsem is None:
            pool_ = self.free_sems.setdefault(E.name, [])
            if pool_:
                semb.dsem, semb.dcnt = pool_.pop()
            else:
                semb.dsem = self.new_sem("d_" + semb.name)
            semb.dcls = E.name
        assert semb.dcls == E.name, (semb.name, semb.dcls, E.name)
        ins = fn(E.eng)
        semb.dcnt += 16
        ev = (semb.dsem, semb.dcnt)
        ins.then_inc(ev[0], 16)
        self._commit(ev, reads, writes)
        for b in adds:
            b.w[id(ev[0])] = ev
        return ins

    def barrier_all(self, bufs):
        self._deps(self.sp, bufs, bufs)

    def full_barrier(self):
        evs = []
        for F in self.engs:
            if F.n > 0:
                ep, off = divmod(F.n - 1, EPOCH)
                evs.append((F.sems[ep], off + 1))
        for b in self.dbufs:
            if b.dsem is not None and b.dcnt > 0:
                evs.append((b.dsem, b.dcnt))
        for E in self.engs:
            for sem, c in evs:
                k = id(sem)
                if E.seen.get(k, 0) < c:
                    E.eng.wait_ge(sem, c)
                    E.seen[k] = c


HORD = [0, 2, 4, 6, 1, 3, 5, 7]
D = 1024
NQ = 29
FMW = NQ * 128
INW = 4352
ALPHA = 2.0 ** 0.25
NEG = -30000.0


def build(S, debug=False, stop_after=None):
    NT = S // 128
    NS = S // 512
    CAP = S // 8
    NJ = CAP // 128
    nc = bass.Bass("TRN2", target_bir_lowering=False)
    P = Prog(nc)
    P.dbufs = []

    def B(name):
        b = Buf(name)
        P.dbufs.append(b)
        return b

    def din(name, shape, dt=F32):
        return nc.dram_tensor(name, list(shape), dt, kind="ExternalInput").ap()

    def dscr(name, shape, dt):
        return nc.dram_tensor(name, list(shape), dt, kind="ExternalOutput" if debug else "Internal").ap()

    xT_d = din("xT", [D, S])
    x_d = din("x", [S, D])
    win_d = din("w_in_p", [D, INW])
    bfm_d = din("bfm", [128, NQ])
    bv_d = din("bv", [128, 640])
    nabi_d = din("nab_int", [128, 40 * 128])
    nabe_d = din("nab_edge", [4, 128, 32 * 128])
    wgb_d = din("wgb", [128, 24 * 128])
    sink_d = din("sinkb", [128, 8])
    wa_d = din("w_a", [512, D])
    wb_d = din("w_b", [512, D])
    wo_d = din("w_o", [D, D])
    wr_d = din("w_r", [D, 16])
    ln1g_d = din("ln1g", [128, D]); ln1b_d = din("ln1b", [128, D])
    ln2g_d = din("ln2g", [128, D]); ln2b_d = din("ln2b", [128, D])
    wg_d = din("w_gate", [16, D, 2048])
    wu_d = din("w_up", [16, D, 2048])
    wd_d = din("w_down", [16, 2048, D])
    bd_d = din("bd_c", [128, 128]); lt_d = din("lt_c", [128, 128])
    out_d = nc.dram_tensor("out", [S, D], F32, kind="ExternalOutput").ap()

    pfm_d = dscr("pfm", [NT, 128, FMW], BF16)
    vtm_d = dscr("vtm", [S, 640], BF16)
    h16_d = dscr("h16", [S, D], BF16)
    acc_d = dscr("acc", [S, D], F32)
    affT_d = dscr("affT", [16, S], F32)
    C_d = dscr("Ccum", [16, S], F32)

    banks = [P.ps(f"bank{i}", [128, 512], F32) for i in range(8)]
    bbank = [B(f"bank{i}") for i in range(8)]
    rot = {"i": 0, "n": 8}

    busy = [False] * 8
    rot["strict"] = False

    def nbank():
        for _ in range(rot["n"]):
            i = rot["i"] % rot["n"]
            rot["i"] += 1
            if not busy[i]:
                break
        else:
            raise RuntimeError("no free PSUM bank")
        if rot["strict"]:
            busy[i] = True
        return banks[i], bbank[i]

    def rel(bb):
        busy[bbank.index(bb)] = False

    identb = P.sb("identb", [128, 128], BF16)
    identf = P.sb("identf", [128, 128], F32)
    b_id = B("ident")
    P.op(P.pool, lambda e: e.memset(identf[:], 0.0), writes=[b_id])
    P.op(P.pool, lambda e: e.affine_select(out=identf[:], in_=identf[:], pattern=[[-1, 128]],
                                           compare_op=ALU.not_equal, fill=1.0, base=0,
                                           channel_multiplier=1), reads=[b_id], writes=[b_id])
    P.op(P.dve, lambda e: e.tensor_copy(out=identb[:], in_=identf[:]), reads=[b_id], writes=[b_id])

    LNS = 12
    stats_l = [P.sb(f"stats{i}", [128, 2, 6], F32) for i in range(LNS)]; b_stats_l = [B(f"stats{i}") for i in range(LNS)]
    mv_l = [P.sb(f"mv{i}", [128, 2], F32) for i in range(LNS)]; b_mv_l = [B(f"mv{i}") for i in range(LNS)]
    lnv_l = [P.sb(f"lnv{i}", [128, 4], F32) for i in range(LNS)]; b_lnv_l = [B(f"lnv{i}") for i in range(LNS)]
    lnc = {"i": 0}
    mhalf = P.sb("mhalf", [128, 1], F32); b_mhalf = B("mhalf")
    P.op(P.pool, lambda e: e.memset(mhalf[:], -0.5), writes=[b_mhalf])
    mone = P.sb("mone", [128, 1], F32)
    P.op(P.pool, lambda e: e.memset(mone[:], -1.0), writes=[b_mhalf])
    idx_all = P.sb("idx_all", [128, 16, NJ], I32); gates = P.sb("gates", [128, 16, NJ], F32)
    b_idx = B("idx_all"); b_gates = B("gates")
    B_h16 = B("h16d"); B_acc = B("accd"); B_affT = B("affTd"); B_pfm = B("pfm"); B_vtm = B("vtm")

    scopes = []

    def push():
        scopes.append((len(P._keep), len(P.dbufs)))

    def pop():
        P.full_barrier()
        n, nb = scopes.pop()
        while len(P._keep) > n:
            P._keep.pop().__exit__(None, None, None)
        for b in P.dbufs[nb:]:
            if b.dsem is not None:
                P.free_sems.setdefault(b.dcls, []).append((b.dsem, b.dcnt))
                b.dsem = None
        del P.dbufs[nb:]

    push()
    W = P.sb("W_in", [128, 8, INW], BF16); b_W = B("W_in")
    win_v = win_d.rearrange("(k p) n -> p k n", p=128)
    xs = [P.sb(f"xs{i}", [128, 8, 512], BF16) for i in range(2)]
    b_xs = [B(f"xs{i}") for i in range(2)]
    xT_v = xT_d.rearrange("(k p) n -> p k n", p=128)

    def a1_load(s):
        P.dma(P.pool, lambda e: e.dma_start(out=xs[s % 2][:], in_=xT_v[:, :, s * 512:(s + 1) * 512]),
              b_xs[s % 2], writes=[b_xs[s % 2]])

    b_Wb_ = []
    for bi in range((INW + 511) // 512):
        c0, c1 = bi * 512, min(INW, bi * 512 + 512)
        bb = B(f"Wld{bi}")
        P.dma(P.pool, lambda e: e.dma_start(out=W[:, :, c0:c1], in_=win_v[:, :, c0:c1]), bb, writes=[bb])
        b_Wb_.append(bb)
        if bi == 0:
            a1_load(0)

    def wdeps(c0, c1):
        return [b_Wb_[i] for i in range(c0 // 512, (c1 - 1) // 512 + 1)]
    bfm = P.sb("bfm", [128, NQ], F32); bq8 = P.sb("bq8", [128, 8], F32); b_bfm = B("bfm")
    bv = P.sb("bv", [128, 640], F32); b_bv = B("bv")
    P.dma(P.sp, lambda e: e.dma_start(out=bfm[:], in_=bfm_d[:, :]), b_bfm, writes=[b_bfm])
    P.dma(P.sp, lambda e: e.dma_start(out=bv[:], in_=bv_d[:, :]), b_bv, writes=[b_bv])
    b_bq8 = B("bq8")
    P.op(P.dve, lambda e: e.tensor_scalar(out=bq8[:], in0=bfm[:, 0:8], scalar1=0.125, scalar2=None,
                                          op0=ALU.mult), reads=[b_bfm], writes=[b_bq8])
    stage = [P.sb(f"stage{i}", [128, 4, NQ, 128], BF16) for i in range(2)]
    b_stage = [B(f"stage{i}") for i in range(2)]
    vst = [P.sb(f"vst{i}", [128, 4, 640], BF16) for i in range(2)]
    b_vst = [B(f"vst{i}") for i in range(2)]
    for s in range(NS):
        if s + 1 < NS:
            a1_load(s + 1)
        xb, bx = xs[s % 2], b_xs[s % 2]
        st, bst = stage[s % 2], b_stage[s % 2]
        vs, bvs = vst[s % 2], b_vst[s % 2]
        for c in range(NQ):
            bk, bbk = nbank()
            for k in range(8):
                P.op(P.pe, lambda e: e.matmul(bk[:, 0:512], lhsT=W[:, k, c * 128:(c + 1) * 128],
                                              rhs=xb[:, k, :], start=(k == 0), stop=(k == 7)),
                     reads=wdeps(c * 128, c * 128 + 128) + [bx], writes=[bbk])
            src = bk[:, 0:512].rearrange("p (t n) -> p t n", n=128)
            if c < 8:
                P.op(P.act, lambda e: e.activation(out=st[:, :, c, :], in_=src, func=AF.Identity,
                                                   bias=bq8[:, c:c + 1], scale=0.125),
                     reads=[bbk, b_bq8], writes=[bst])
            elif c < 24:
                P.op(P.act, lambda e: e.activation(out=st[:, :, c, :], in_=src, func=AF.Sigmoid,
                                                   bias=bfm[:, c:c + 1], scale=1.0),
                     reads=[bbk, b_bfm], writes=[bst])
            else:
                P.op(P.act, lambda e: e.activation(out=st[:, :, c, :], in_=src, func=AF.Identity,
                                                   bias=bfm[:, c:c + 1], scale=1.0),
                     reads=[bbk, b_bfm], writes=[bst])
        for i in range(4):
            bk, bbk = nbank()
            for k in range(8):
                P.op(P.pe, lambda e: e.matmul(bk[:, 0:512], lhsT=xb[:, k, i * 128:(i + 1) * 128],
                                              rhs=W[:, k, FMW:FMW + 512], start=(k == 0), stop=(k == 7)),
                     reads=wdeps(FMW, FMW + 512) + [bx], writes=[bbk])
            P.op(P.dve, lambda e: e.tensor_tensor(out=vs[:, i, 0:512], in0=bk[:, 0:512], in1=bv[:, 0:512],
                                                  op=ALU.add), reads=[bbk, b_bv], writes=[bvs])
            bk, bbk = nbank()
            for k in range(8):
                P.op(P.pe, lambda e: e.matmul(bk[:, 0:128], lhsT=xb[:, k, i * 128:(i + 1) * 128],
                                              rhs=W[:, k, FMW + 512:INW], start=(k == 0), stop=(k == 7)),
                     reads=wdeps(FMW + 512, INW) + [bx], writes=[bbk])
            P.op(P.dve, lambda e: e.tensor_tensor(out=vs[:, i, 512:640], in0=bk[:, 0:128], in1=bv[:, 512:640],
                                                  op=ALU.add), reads=[bbk, b_bv], writes=[bvs])
        P.dma(P.sp, lambda e: e.dma_start(
            out=pfm_d[4 * s:4 * s + 4].rearrange("t p f -> p t f"),
            in_=st[:].rearrange("p t c n -> p t (c n)")), bst, reads=[bst], adds=[B_pfm])
        P.dma(P.sp, lambda e: e.dma_start(
            out=vtm_d.rearrange("(t p) f -> p t f", p=128)[:, 4 * s:4 * s + 4, :],
            in_=vs[:]), bvs, reads=[bvs], adds=[B_vtm])
    pop()
    if stop_after == "A1":
        return nc

    push()
    rot["n"] = 8; rot["i"] = 0; rot["strict"] = True
    Wa = P.sb("Wa", [128, 4, D], BF16); Wb = P.sb("Wb", [128, 4, D], BF16); Wo = P.sb("Wo", [128, 8, D], BF16)
    Wr = P.sb("Wr", [128, 8, 16], F32)
    b_Wa = B("Wa"); b_Wb = B("Wb"); b_Wo = B("Wo"); b_Wr = B("Wr")
    def load_merge_w():
        P.dma(P.pool, lambda e: e.dma_start(out=Wa[:], in_=wa_d.rearrange("(k p) n -> p k n", p=128)), b_Wa, writes=[b_Wa])
        P.dma(P.pool, lambda e: e.dma_start(out=Wb[:], in_=wb_d.rearrange("(k p) n -> p k n", p=128)), b_Wb, writes=[b_Wb])
        P.dma(P.pool, lambda e: e.dma_start(out=Wo[:], in_=wo_d.rearrange("(k p) n -> p k n", p=128)), b_Wo, writes=[b_Wo])
    P.dma(P.sp, lambda e: e.dma_start(out=Wr[:], in_=wr_d.rearrange("(k p) n -> p k n", p=128)), b_Wr, writes=[b_Wr])
    nabi = P.sb("nabi", [128, 40 * 128], BF16); b_nabi = B("nabi")
    nabe = P.sb("nabe", [128, 32 * 128], BF16); b_nabe = B("nabe")
    wgb = P.sb("wgb", [128, 24 * 128], BF16); b_wgb = B("wgb")
    for c0 in range(0, 40 * 128, 2048):
        c1 = min(c0 + 2048, 40 * 128)
        P.dma(P.pool, lambda e: e.dma_start(out=nabi[:, c0:c1], in_=nabi_d[:, c0:c1]), B(f"nabi{c0}"), adds=[b_nabi])
    for c0 in range(0, 24 * 128, 1536):
        P.dma(P.pool, lambda e: e.dma_start(out=wgb[:, c0:c0 + 1536], in_=wgb_d[:, c0:c0 + 1536]), B(f"wgb{c0}"), adds=[b_wgb])
    ln1g = P.sb("ln1g", [128, D], F32); ln1b = P.sb("ln1b", [128, D], F32); b_ln1 = B("ln1")
    P.dma(P.sp, lambda e: e.dma_start(out=ln1g[:], in_=ln1g_d[:, :]), B("ln1g"), adds=[b_ln1])
    P.dma(P.sp, lambda e: e.dma_start(out=ln1b[:], in_=ln1b_d[:, :]), B("ln1b"), adds=[b_ln1])
    esink = P.sb("esink", [128, 8, 1], F32); b_esink = B("esink")
    P.dma(P.sp, lambda e: e.dma_start(out=esink[:].rearrange("p h o -> p (h o)"), in_=sink_d[:, :]), b_esink, writes=[b_esink])
    P.op(P.act, lambda e: e.activation(out=esink[:], in_=esink[:], func=AF.Exp), reads=[b_esink], writes=[b_esink])

    Q2 = [P.sb(f"Q2_{i}", [128, 8, 128], BF16) for i in range(2)]; b_Q2 = [B(f"Q2_{i}") for i in range(2)]
    G4 = [P.sb(f"G4_{i}", [128, 16, 128], BF16) for i in range(4)]; b_G4 = [B(f"G4_{i}") for i in range(4)]
    Kr = [P.sb(f"Kr{i}", [128, 5, 128], BF16) for i in range(8)]; b_K = [B(f"Kr{i}") for i in range(8)]
    Vr = [P.sb(f"Vr{i}", [128, 10, 65], BF16) for i in range(8)]; b_V = [B(f"Vr{i}") for i in range(8)]
    for i in range(8):
        P.op(P.pool, lambda e: e.memset(Vr[i][:, :, 64:65], 1.0), writes=[b_V[i]])
    xtok = [P.sb(f"xtok{i}", [128, D], F32) for i in range(2)]; b_xtok = [B(f"xtok{i}") for i in range(2)]
    sc = [P.sb(f"sc{i}", [128, 512], F32) for i in range(3)]; b_sc = [B(f"sc{i}") for i in range(3)]
    PTa2 = [P.sb(f"PTa{i}", [128, 40, 128], BF16) for i in range(2)]; b_PTa2 = [B(f"PTa{i}") for i in range(2)]
    PTb2 = [P.sb(f"PTb{i}", [128, 3, 2, 4, 128], BF16) for i in range(2)]; b_PTb2 = [B(f"PTb{i}") for i in range(2)]
    rden = P.sb("rden", [128, 16, 1], F32); b_rden = B("rden")
    ya = P.sb("ya", [128, 8, 64], BF16); yb = P.sb("yb", [128, 8, 64], BF16); b_ya = B("ya"); b_yb = B("yb")
    yT2 = [P.sb(f"yT{i}", [128, 8, 128], BF16) for i in range(2)]; b_yT2 = [B(f"yT{i}") for i in range(2)]
    t1 = P.sb("t1", [128, 512], F32); t2 = P.sb("t2", [128, 512], F32); b_t1 = B("t1"); b_t2 = B("t2")
    mixT2 = [P.sb(f"mixT{i}", [128, 8, 128], BF16) for i in range(2)]; b_mixT2 = [B(f"mixT{i}") for i in range(2)]
    rr = [P.sb(f"rr{i}", [128, D], F32) for i in range(2)]; b_rr = [B(f"rr{i}") for i in range(2)]
    hh = [P.sb(f"hh{i}", [128, D], F32) for i in range(2)]; b_hh = [B(f"hh{i}") for i in range(2)]
    h16 = [P.sb(f"h16_{i}", [128, D], BF16) for i in range(2)]; b_h16s = [B(f"h16_{i}") for i in range(2)]
    hT = P.sb("hT", [128, 8, 128], F32); b_hT = B("hT")
    sm = P.sb("sm", [128, 4], F32); b_sm = B("sm")
    e16 = P.sb("e16", [128, 16], F32); aff = P.sb("aff", [128, 16], F32); b_e16 = B("e16"); b_aff = B("aff")
    affTs = [P.sb(f"affTs{i}", [16, 128], F32) for i in range(2)]; b_affTs = [B(f"affTs{i}") for i in range(2)]
    cnt = {"sc": 0}
    edge_tiles = [0, 1, NT - 2, NT - 1]

    def load_kv(kt):
        sl = kt % 8
        P.dma(P.sp, lambda e: e.dma_start(out=Kr[sl][:].rearrange("p c n -> p (c n)"), in_=pfm_d[kt, :, 3072:FMW]),
              b_K[sl], reads=[B_pfm], writes=[b_K[sl]])
        P.dma(P.sp, lambda e: e.dma_start(out=Vr[sl][:, :, 0:64],
                                          in_=vtm_d[kt * 128:(kt + 1) * 128, :].rearrange("p (h d) -> p h d", d=64)),
              b_V[sl], reads=[B_vtm], writes=[b_V[sl]])

    def load_q(t):
        sl = t % 2
        P.dma(P.sp, lambda e: e.dma_start(out=Q2[sl][:].rearrange("p c n -> p (c n)"), in_=pfm_d[t, :, 0:1024]),
              b_Q2[sl], reads=[B_pfm], writes=[b_Q2[sl]])
        g = t % 4
        P.dma(P.sp, lambda e: e.dma_start(out=G4[g][:].rearrange("p c n -> p (c n)"), in_=pfm_d[t, :, 1024:3072]),
              b_G4[g], reads=[B_pfm], writes=[b_G4[g]])

    def load_x(t):
        sl = t % 2
        P.dma(P.sp, lambda e: e.dma_start(out=xtok[sl][:], in_=x_d[t * 128:(t + 1) * 128, :]),
              b_xtok[sl], writes=[b_xtok[sl]])

    def stage1(t):
        kts = _kts(t, NT); nk = len(kts)
        q = Q2[t % 2]; bq = b_Q2[t % 2]
        PTa = PTa2[t % 2]; b_PTa = b_PTa2[t % 2]; PTb = PTb2[t % 2]; b_PTb = b_PTb2[t % 2]
        if nk == 5:
            nab, bnab = nabi, b_nabi
        else:
            ei = edge_tiles.index(t)
            for c0 in range(0, 32 * 128, 2048):
                P.dma(P.pool, lambda e: e.dma_start(out=nabe[:, c0:c0 + 2048], in_=nabe_d[ei, :, c0:c0 + 2048]),
                      B(f"nabe{t}_{c0}"), writes=[b_nabe] if c0 == 0 else [], adds=[b_nabe] if c0 else [])
            nab, bnab = nabe, b_nabe
        for uu in range(2 * nk):
            u = (uu // 2) + nk * (uu % 2)
            bk, bbk = nbank()
            for jj in range(4):
                hp, i = divmod(4 * u + jj, nk); h = HORD[hp]; kt = kts[i]; ch, half = divmod(h, 2)
                sl = slice(64 * half, 64 * half + 64)
                P.op(P.pe, lambda e: e.matmul(bk[:, jj * 128:(jj + 1) * 128], lhsT=Kr[kt % 8][sl, ch, :],
                                              rhs=q[sl, ch, :], start=True, stop=True),
                     reads=[b_K[kt % 8], bq], writes=[bbk])
            si = cnt["sc"] % 3; cnt["sc"] += 1
            P.op(P.dve, lambda e: e.tensor_tensor(out=sc[si][:], in0=bk[:, 0:512], in1=nab[:, u * 512:(u + 1) * 512],
                                                  op=ALU.add), reads=[bbk, bnab], writes=[b_sc[si]])
            rel(bbk)
            P.op(P.act, lambda e: e.activation(out=PTa[:, 4 * u:4 * u + 4, :].rearrange("p b n -> p (b n)"),
                                               in_=sc[si][:], func=AF.Exp), reads=[b_sc[si]], writes=[b_PTa])
            yield
        dts = [dt for dt in (-1, 0, 1) if 0 <= t + dt < NT]
        for j in range(2):
            sl = slice(64 * j, 64 * j + 64)
            for dt in dts:
                di = dt + 1; kt = t + dt
                bk, bbk = nbank()
                P.op(P.pe, lambda e: e.matmul(bk[:, 0:512], lhsT=Kr[kt % 8][sl, 4, :],
                                              rhs=q[sl, 4:8, :].rearrange("p c n -> p (c n)"), start=True, stop=True),
                     reads=[b_K[kt % 8], bq], writes=[bbk])
                si = cnt["sc"] % 3; cnt["sc"] += 1
                o = (di * 2 + j) * 512
                P.op(P.dve, lambda e: e.tensor_tensor(out=sc[si][:], in0=bk[:, 0:512], in1=wgb[:, o:o + 512],
                                                      op=ALU.add), reads=[bbk, b_wgb], writes=[b_sc[si]])
                rel(bbk)
                P.op(P.act, lambda e: e.activation(out=PTb[:, di, j].rearrange("p g n -> p (g n)"),
                                                   in_=sc[si][:], func=AF.Exp), reads=[b_sc[si]], writes=[b_PTb])
                yield

    def s2_pv(t):
        kts = _kts(t, NT); nk = len(kts)
        PTa = PTa2[t % 2]; b_PTa = b_PTa2[t % 2]; PTb = PTb2[t % 2]; b_PTb = b_PTb2[t % 2]
        dts = [dt for dt in (-1, 0, 1) if 0 <= t + dt < NT]
        for hb in range(2):
            pv, bpv = nbank()
            for h in range(4 * hb, 4 * hb + 4):
                col = (h % 4) * 65
                for i, kt in enumerate(kts):
                    P.op(P.pe, lambda e: e.matmul(pv[:, col:col + 65], lhsT=PTa[:, HORD.index(h) * nk + i, :],
                                                  rhs=Vr[kt % 8][:, h, :], start=(i == 0), stop=(i == nk - 1)),
                         reads=[b_PTa, b_V[kt % 8]], writes=[bpv])
                yield
            pvv = pv[:, 0:260].rearrange("p (h d) -> p h d", d=65)
            P.op(P.dve, lambda e: e.reciprocal(out=rden[:, 4 * hb:4 * hb + 4, :], in_=pvv[:, :, 64:65]),
                 reads=[bpv], writes=[b_rden])
            P.op(P.dve, lambda e: e.tensor_tensor(out=ya[:, 4 * hb:4 * hb + 4, :], in0=pvv[:, :, 0:64],
                                                  in1=rden[:, 4 * hb:4 * hb + 4, :].to_broadcast([128, 4, 64]),
                                                  op=ALU.mult), reads=[bpv, b_rden], writes=[b_ya])
            rel(bpv)
        for hb in range(2):
            pv, bpv = nbank()
            for hd in range(4 * hb, 4 * hb + 4):
                j, g = divmod(hd, 4); col = (hd % 4) * 65
                for ii, dt in enumerate(dts):
                    kt = t + dt
                    P.op(P.pe, lambda e: e.matmul(pv[:, col:col + 65], lhsT=PTb[:, dt + 1, j, g, :],
                                                  rhs=Vr[kt % 8][:, 8 + j, :], start=(ii == 0), stop=(ii == len(dts) - 1)),
                         reads=[b_PTb, b_V[kt % 8]], writes=[bpv])
                yield
            pvv = pv[:, 0:260].rearrange("p (h d) -> p h d", d=65)
            rs = rden[:, 8 + 4 * hb:12 + 4 * hb, :]
            P.op(P.dve, lambda e: e.tensor_tensor(out=rs, in0=pvv[:, :, 64:65], in1=esink[:, 4 * hb:4 * hb + 4, :],
                                                  op=ALU.add), reads=[bpv, b_esink], writes=[b_rden])
            P.op(P.dve, lambda e: e.reciprocal(out=rs, in_=rs), reads=[b_rden], writes=[b_rden])
            P.op(P.dve, lambda e: e.tensor_tensor(out=yb[:, 4 * hb:4 * hb + 4, :], in0=pvv[:, :, 0:64],
                                                  in1=rs.to_broadcast([128, 4, 64]), op=ALU.mult),
                 reads=[bpv, b_rden], writes=[b_yb])
            rel(bpv)

    def s2_tr(t):
        yT = yT2[t % 2]; b_yT = b_yT2[t % 2]
        bk, bbk = nbank()
        bkb = bk[:].bitcast(BF16)
        yaf = ya[:].rearrange("p h d -> p (h d)"); ybf = yb[:].rearrange("p h d -> p (h d)")
        for c in range(4):
            P.op(P.pe, lambda e: e.transpose(out=bkb[:, c * 128:(c + 1) * 128], in_=yaf[:, c * 128:(c + 1) * 128],
                                             identity=identb[:]), reads=[b_ya, b_id], writes=[bbk])
        for c in range(4):
            P.op(P.pe, lambda e: e.transpose(out=bkb[:, 512 + c * 128:512 + (c + 1) * 128],
                                             in_=ybf[:, c * 128:(c + 1) * 128], identity=identb[:]),
                 reads=[b_yb, b_id], writes=[bbk])
        P.op(P.act, lambda e: e.copy(out=yT[:].rearrange("p c n -> p (c n)"), in_=bkb[:, 0:1024]),
             reads=[bbk], writes=[b_yT])
        rel(bbk)
        yield

    def s2_br(t):
        yT = yT2[t % 2]; b_yT = b_yT2[t % 2]
        mixT = mixT2[t % 2]; b_mixT = b_mixT2[t % 2]
        g4 = G4[t % 4]; bq = b_G4[t % 4]
        for half in range(2):
            bkX, bbX = nbank()
            for c in range(4):
                for k in range(4):
                    P.op(P.pe, lambda e: e.matmul(bkX[:, c * 128:(c + 1) * 128],
                                                  lhsT=Wa[:, k, (4 * half + c) * 128:(4 * half + c + 1) * 128],
                                                  rhs=yT[:, k, :], start=(k == 0), stop=(k == 3)),
                         reads=[b_Wa, b_yT], writes=[bbX])
            yield
            bkY, bbY = nbank()
            for c in range(4):
                for k in range(4):
                    P.op(P.pe, lambda e: e.matmul(bkY[:, c * 128:(c + 1) * 128],
                                                  lhsT=Wb[:, k, (4 * half + c) * 128:(4 * half + c + 1) * 128],
                                                  rhs=yT[:, 4 + k, :], start=(k == 0), stop=(k == 3)),
                         reads=[b_Wb, b_yT], writes=[bbY])
            yield
            P.op(P.dve, lambda e: e.tensor_tensor(out=t1[:], in0=bkX[:, 0:512],
                                                  in1=g4[:, 4 * half:4 * half + 4, :].rearrange("p c n -> p (c n)"),
                                                  op=ALU.mult), reads=[bbX, bq], writes=[b_t1])
            rel(bbX)
            P.op(P.dve, lambda e: e.tensor_tensor(out=t2[:], in0=bkY[:, 0:512],
                                                  in1=g4[:, 8 + 4 * half:12 + 4 * half, :].rearrange("p c n -> p (c n)"),
                                                  op=ALU.mult), reads=[bbY, bq], writes=[b_t2])
            rel(bbY)
            P.op(P.pool, lambda e: e.tensor_tensor(out=mixT[:, 4 * half:4 * half + 4, :].rearrange("p c n -> p (c n)"),
                                                   in0=t1[:], in1=t2[:], op=ALU.add),
                 reads=[b_t1, b_t2], writes=[b_mixT])

    def s2_mix_b(t):
        mixT = mixT2[t % 2]; b_mixT = b_mixT2[t % 2]
        r = rr[t % 2]; br = b_rr[t % 2]
        xt = xtok[t % 2]; bxt = b_xtok[t % 2]
        for chh in range(2):
            bk, bbk = nbank()
            for k in range(8):
                P.op(P.pe, lambda e: e.matmul(bk[:, 0:512], lhsT=mixT[:, k, :], rhs=Wo[:, k, chh * 512:(chh + 1) * 512],
                                              start=(k == 0), stop=(k == 7)), reads=[b_mixT, b_Wo], writes=[bbk])
            P.op(P.dve, lambda e: e.scalar_tensor_tensor(out=r[:, chh * 512:(chh + 1) * 512],
                                                         in0=xt[:, chh * 512:(chh + 1) * 512], scalar=ALPHA,
                                                         in1=bk[:, 0:512], op0=ALU.mult, op1=ALU.add),
                 reads=[bbk, bxt], writes=[br])
            rel(bbk)
            yield

    def s2_ln(t):
        r = rr[t % 2]; br = b_rr[t % 2]; h = hh[t % 2]; bh = b_hh[t % 2]
        layer_norm(r, br, h, bh, ln1g, ln1b, b_ln1, g_on_pool=True)
        h6 = h16[t % 2]; bh6 = b_h16s[t % 2]
        P.op(P.act, lambda e: e.copy(out=h6[:], in_=h[:]), reads=[bh], writes=[bh6])
        P.op(P.act, lambda e: e.activation(out=r[:], in_=h[:], func=AF.Identity, scale=ALPHA), reads=[bh], writes=[br])
        P.dma(P.sp, lambda e: e.dma_start(out=h16_d[t * 128:(t + 1) * 128, :], in_=h6[:]), bh6, reads=[bh6], adds=[B_h16])
        P.dma(P.sp, lambda e: e.dma_start(out=acc_d[t * 128:(t + 1) * 128, :], in_=r[:]), br, reads=[br], adds=[B_acc])

    def s2_ln_b(t):
        h = hh[t % 2]; bh = b_hh[t % 2]
        for kb in range(2):
            bk, bbk = nbank()
            for k4 in range(4):
                k = 4 * kb + k4
                P.op(P.pe, lambda e: e.transpose(out=bk[:, k4 * 128:(k4 + 1) * 128], in_=h[:, k * 128:(k + 1) * 128],
                                                 identity=identf[:]), reads=[bh, b_id], writes=[bbk])
            P.op(P.act, lambda e: e.copy(out=hT[:, 4 * kb:4 * kb + 4, :].rearrange("p c n -> p (c n)"), in_=bk[:, 0:512]),
                 reads=[bbk], writes=[b_hT])
            rel(bbk)
            yield
        bk, bbk = nbank()
        for k in range(8):
            P.op(P.pe, lambda e: e.matmul(bk[:, 0:16], lhsT=hT[:, k, :], rhs=Wr[:, k, :], start=(k == 0), stop=(k == 7)),
                 reads=[b_hT, b_Wr], writes=[bbk])
        yield
        P.op(P.act, lambda e: e.activation(out=e16[:], in_=bk[:, 0:16], func=AF.Exp, accum_out=sm[:, 2:3]),
             reads=[bbk], writes=[b_e16, b_sm])
        rel(bbk)
        P.op(P.pool, lambda e: e.tensor_tensor(out=sm[:, 3:4], in0=sm[:, 2:3], in1=mone[:], op=ALU.pow),
             reads=[b_sm, b_mhalf], writes=[b_sm])
        P.op(P.pool, lambda e: e.tensor_scalar(out=aff[:], in0=e16[:], scalar1=sm[:, 3:4], scalar2=1.0,
                                               op0=ALU.mult, op1=ALU.mult), reads=[b_e16, b_sm], writes=[b_aff])
        bk, bbk = nbank()
        P.op(P.pe, lambda e: e.transpose(out=bk[0:16, 0:128], in_=aff[:, 0:16], identity=identf[:]),
             reads=[b_aff, b_id], writes=[bbk])
        ats = affTs[t % 2]; bats = b_affTs[t % 2]
        P.op(P.act, lambda e: e.copy(out=ats[:], in_=bk[0:16, 0:128]), reads=[bbk], writes=[bats])
        rel(bbk)
        P.dma(P.sp, lambda e: e.dma_start(out=affT_d[:, t * 128:(t + 1) * 128], in_=ats[:]), bats, reads=[bats], adds=[B_affT])

    def layer_norm(r, br, h, bh, g, b, bgb, g_on_pool=False, split=False):
        li = lnc["i"] % LNS; lnc["i"] += 1
        stats, mv, lnv = stats_l[li], mv_l[li], lnv_l[li]
        b_stats, b_mv, b_lnv = b_stats_l[li], b_mv_l[li], b_lnv_l[li]
        for c in range(2):
            P.op(P.dve, lambda e: e.bn_stats(out=stats[:, c, :], in_=r[:, c * 512:(c + 1) * 512]), reads=[br], writes=[b_stats])
        P.op(P.dve, lambda e: e.bn_aggr(out=mv[:], in_=stats[:].rearrange("p a b -> p (a b)")), reads=[b_stats], writes=[b_mv])
        P.op(P.dve, lambda e: e.tensor_scalar(out=lnv[:, 0:1], in0=mv[:, 1:2], scalar1=1e-5, scalar2=None, op0=ALU.add),
             reads=[b_mv], writes=[b_lnv])
        P.op(P.pool, lambda e: e.tensor_tensor(out=lnv[:, 1:2], in0=lnv[:, 0:1], in1=mhalf[:], op=ALU.pow),
             reads=[b_lnv, b_mhalf], writes=[b_lnv])
        P.op(P.pool, lambda e: e.tensor_scalar(out=lnv[:, 2:3], in0=mv[:, 0:1], scalar1=-1.0, scalar2=lnv[:, 1:2],
                                               op0=ALU.mult, op1=ALU.mult), reads=[b_mv, b_lnv], writes=[b_lnv])
        P.op(P.act, lambda e: e.activation(out=h[:], in_=r[:], func=AF.Identity, bias=lnv[:, 2:3], scale=lnv[:, 1:2]),
             reads=[br, b_lnv], writes=[bh])
        if not split:
            ln_affine(h, bh, g, b, bgb, g_on_pool)

    def ln_affine(h, bh, g, b, bgb, g_on_pool=False):
        ge_ = P.pool if g_on_pool else P.dve
        P.op(ge_, lambda e: e.tensor_tensor(out=h[:], in0=h[:], in1=g[:], op=ALU.mult), reads=[bh, bgb], writes=[bh])
        P.op(P.pool, lambda e: e.tensor_tensor(out=h[:], in0=h[:], in1=b[:], op=ALU.add), reads=[bh, bgb], writes=[bh])

    import itertools

    def drive(gA, gBs):
        gBs = list(gBs)
        aA = gA is not None
        while aA or gBs:
            if aA:
                try:
                    next(gA)
                except StopIteration:
                    aA = False
            for g in list(gBs):
                try:
                    next(g)
                except StopIteration:
                    gBs.remove(g)

    for kt in range(min(5, NT)):
        load_kv(kt)
    load_q(0)
    drive(stage1(0), [])
    load_merge_w()
    for t in range(NT + 4):
        if t + 5 < NT:
            load_kv(t + 5)
        if t + 1 < NT:
            load_q(t + 1)
        if 0 <= t - 1 < NT:
            load_x(t - 1)
        if 0 <= t - 3 < NT:
            s2_ln(t - 3)
        gens = []
        if t < NT:
            gens.append(itertools.chain(s2_pv(t), s2_tr(t)))
        if 0 <= t - 1 < NT:
            gens.append(s2_br(t - 1))
        if 0 <= t - 2 < NT:
            gens.append(s2_mix_b(t - 2))
        if 0 <= t - 4 < NT:
            gens.append(s2_ln_b(t - 4))
        drive(stage1(t + 1) if t + 1 < NT else None, gens)
    pop()
    rot["n"] = 8; rot["strict"] = False
    if stop_after == "A2":
        return nc

    push()
    Wg_r = [P.sb(f"Wg{i}", [128, 8, 512], BF16) for i in range(3)]; b_Wg = [B(f"Wg{i}") for i in range(3)]
    Wu_r = [P.sb(f"Wu{i}", [128, 8, 512], BF16) for i in range(3)]; b_Wu = [B(f"Wu{i}") for i in range(3)]
    Wd_r = [P.sb(f"Wd{i}", [128, 4, D], BF16) for i in range(4)]; b_Wd = [B(f"Wd{i}") for i in range(4)]
    def load_gu(n):
        ex, fb = divmod(n, 4); sl = n % 3
        P.dma(P.pool, lambda e: e.dma_start(out=Wg_r[sl][:], in_=wg_d[ex].rearrange("(k p) n -> p k n", p=128)[:, :, fb * 512:(fb + 1) * 512]),
              b_Wg[sl], writes=[b_Wg[sl]])
        P.dma(P.pool, lambda e: e.dma_start(out=Wu_r[sl][:], in_=wu_d[ex].rearrange("(k p) n -> p k n", p=128)[:, :, fb * 512:(fb + 1) * 512]),
              b_Wu[sl], writes=[b_Wu[sl]])

    def load_d(ex):
        for fb in range(4):
            P.dma(P.pool, lambda e: e.dma_start(out=Wd_r[fb][:], in_=wd_d[ex, fb * 512:(fb + 1) * 512, :].rearrange("(c p) n -> p c n", p=128)),
                  b_Wd[fb], writes=[b_Wd[fb]])

    load_gu(0); load_gu(1); load_d(0)

    push()
    G8 = 8; SG = S // G8
    affS = P.sb("affS", [128, SG], F32); b_affS = B("affS")
    P.dma(P.sp, lambda e: e.dma_start(out=affS[:], in_=affT_d.rearrange("e (g n) -> (e g) n", g=G8)), b_affS,
          reads=[B_affT], writes=[b_affS])
    junk = P.sb("junk", [128, SG], F32); b_junk = B("junk")
    onesS = P.sb("onesS", [128, SG], F32); b_ones = B("onesS")
    Cs = P.sb("Cs", [128, SG], F32); b_Cs = B("Cs")
    bis = P.sb("bis", [128, 8], F32); b_bis = B("bis")
    BD = P.sb("BD", [128, 128], F32); LT = P.sb("LT", [128, 128], F32); b_BD = B("BD")
    P.dma(P.sp, lambda e: e.dma_start(out=BD[:], in_=bd_d[:, :]), B("BDl"), adds=[b_BD])
    P.dma(P.sp, lambda e: e.dma_start(out=LT[:], in_=lt_d[:, :]), B("LTl"), adds=[b_BD])
    P.op(P.pool, lambda e: e.memset(onesS[:], 1.0), writes=[b_ones])
    P.op(P.dve, lambda e: e.memset(bis[:, 0:1], 0.0), writes=[b_bis])
    P.op(P.dve, lambda e: e.memset(bis[:, 1:2], 1.0), reads=[b_bis], writes=[b_bis])
    lo, hi, mid, cn, ge, dd = [bis[:, i:i + 1] for i in range(6)]
    for it in range(32):
        P.op(P.dve, lambda e: e.tensor_scalar(out=mid, in0=lo, scalar1=hi, scalar2=0.5, op0=ALU.add, op1=ALU.mult),
             reads=[b_bis], writes=[b_bis])
        P.op(P.dve, lambda e: e.tensor_scalar(out=junk[:], in0=affS[:], scalar1=mid, scalar2=None, op0=ALU.is_ge,
                                              op1=ALU.add, accum_out=cn), reads=[b_bis, b_affS], writes=[b_bis, b_junk])
        bk, bbk = nbank()
        P.op(P.pe, lambda e: e.matmul(bk[:, 0:1], lhsT=BD[:], rhs=cn, start=True, stop=True),
             reads=[b_BD, b_bis], writes=[bbk])
        P.op(P.dve, lambda e: e.tensor_scalar(out=ge, in0=bk[:, 0:1], scalar1=float(CAP) - 0.5, scalar2=None,
                                              op0=ALU.is_ge), reads=[bbk, b_bis], writes=[b_bis])
        P.op(P.dve, lambda e: e.tensor_tensor(out=dd, in0=mid, in1=lo, op=ALU.subtract), reads=[b_bis], writes=[b_bis])
        P.op(P.dve, lambda e: e.scalar_tensor_tensor(out=lo, in0=dd, scalar=ge, in1=lo, op0=ALU.mult, op1=ALU.add),
             reads=[b_bis], writes=[b_bis])
        P.op(P.dve, lambda e: e.tensor_tensor(out=dd, in0=hi, in1=mid, op=ALU.subtract), reads=[b_bis], writes=[b_bis])
        P.op(P.dve, lambda e: e.scalar_tensor_tensor(out=hi, in0=dd, scalar=ge, in1=mid, op0=ALU.mult, op1=ALU.add),
             reads=[b_bis], writes=[b_bis])
    P.op(P.dve, lambda e: e.tensor_scalar(out=junk[:], in0=affS[:], scalar1=lo, scalar2=None, op0=ALU.is_ge),
         reads=[b_bis, b_affS], writes=[b_junk])
    P.op(P.dve, lambda e: e.tensor_tensor_scan(out=Cs[:], data0=onesS[:], data1=junk[:], initial=0.0,
                                               op0=ALU.mult, op1=ALU.add), reads=[b_ones, b_junk], writes=[b_Cs])
    bk, bbk = nbank()
    P.op(P.pe, lambda e: e.matmul(bk[:, 0:1], lhsT=LT[:], rhs=Cs[:, SG - 1:SG], start=True, stop=True),
         reads=[b_BD, b_Cs], writes=[bbk])
    P.op(P.dve, lambda e: e.tensor_copy(out=bis[:, 6:7], in_=bk[:, 0:1]), reads=[bbk, b_bis], writes=[b_bis])
    P.op(P.dve, lambda e: e.tensor_scalar(out=Cs[:], in0=Cs[:], scalar1=bis[:, 6:7], scalar2=None, op0=ALU.add),
         reads=[b_bis, b_Cs], writes=[b_Cs])
    b_Cd = B("Cd")
    P.dma(P.sp, lambda e: e.dma_start(out=C_d.rearrange("e (g n) -> (e g) n", g=G8), in_=Cs[:]), b_Cs,
          reads=[b_Cs], writes=[b_Cd])
    C3 = C_d.rearrange("e (t p) -> t e p", p=128)
    Ct = P.sb("Ct", [NT, 16, 129], F32); b_Ct = B("Ct")
    At = P.sb("At", [NT, 16, 128], F32); b_At = B("At")
    CTp = P.sb("CTp", [NT, 16, 1], F32); b_CTp = B("CTp")
    P.dma(P.sp, lambda e: e.dma_start(out=Ct[:, :, 0:128], in_=C3), b_Ct, reads=[b_Cd], writes=[b_Ct])
    P.op(P.pool, lambda e: e.iota(Ct[:, :, 128:129], pattern=[[0, 16]], base=0, channel_multiplier=128,
                                  allow_small_or_imprecise_dtypes=True), reads=[b_Ct], writes=[b_Ct])
    P.dma(P.sp, lambda e: e.dma_start(out=At[:], in_=affT_d.rearrange("e (t p) -> t e p", p=128)), b_At,
          reads=[B_affT], writes=[b_At])
    P.op(P.dve, lambda e: e.memset(CTp[:], 0.0), writes=[b_CTp])
    P.dma(P.sp, lambda e: e.dma_start(out=CTp[1:NT, :, :], in_=C3[0:NT - 1, :, 127:128], allow_slow_non_contiguous=True), b_CTp,
          reads=[b_Cd], writes=[b_CTp])
    iota_c = P.sb("iota_c", [NT, CAP], F32); cg = P.sb("cg", [128, NJ], F32); iota_p = P.sb("iota_p", [128, 128], F32)
    b_iota = B("iota")
    P.op(P.pool, lambda e: e.iota(iota_c[:], pattern=[[1, CAP]], base=0, channel_multiplier=0,
                                  allow_small_or_imprecise_dtypes=True), writes=[b_iota])
    P.op(P.pool, lambda e: e.iota(cg[:], pattern=[[128, NJ]], base=0, channel_multiplier=1,
                                  allow_small_or_imprecise_dtypes=True), writes=[b_iota])
    P.op(P.pool, lambda e: e.iota(iota_p[:], pattern=[[1, 128]], base=0, channel_multiplier=0,
                                  allow_small_or_imprecise_dtypes=True), writes=[b_iota])
    a_t = P.sb("a_t", [NT, CAP], F32); oh = P.sb("oh", [NT, CAP], F32); b_at = B("a_t"); b_oh = B("oh")
    jk = [P.sb(f"jk{i}", [128, 128], F32) for i in range(2)]; b_jk = [B(f"jk{i}") for i in range(2)]
    nloc = P.sb("nloc", [128, 2], F32); b_nloc = B("nloc")
    for ex in range(16):
        P.op(P.dve, lambda e: e.tensor_scalar(out=a_t[:], in0=iota_c[:], scalar1=CTp[:, ex, :], scalar2=None,
                                              op0=ALU.is_lt), reads=[b_iota, b_CTp], writes=[b_at])
        P.op(P.dve, lambda e: e.scalar_tensor_tensor(out=oh[:], in0=iota_c[:], scalar=Ct[:, ex, 127:128], in1=a_t[:],
                                                     op0=ALU.is_lt, op1=ALU.subtract),
             reads=[b_iota, b_Ct, b_at], writes=[b_oh])
        for j in range(NJ):
            bk, bbk = nbank()
            P.op(P.pe, lambda e: e.matmul(bk[:, 0:129], lhsT=oh[:, j * 128:(j + 1) * 128], rhs=Ct[:, ex, :],
                                          start=True, stop=True), reads=[b_oh, b_Ct], writes=[bbk])
            P.op(P.pe, lambda e: e.matmul(bk[:, 256:384], lhsT=oh[:, j * 128:(j + 1) * 128], rhs=At[:, ex, :],
                                          start=True, stop=True), reads=[b_oh, b_At], writes=[bbk])
            P.op(P.dve, lambda e: e.tensor_scalar(out=jk[0][:], in0=bk[:, 0:128], scalar1=cg[:, j:j + 1], scalar2=None,
                                                  op0=ALU.is_le, op1=ALU.add, accum_out=nloc[:, 0:1]),
                 reads=[bbk, b_iota], writes=[b_jk[0], b_nloc])
            P.op(P.dve, lambda e: e.tensor_tensor(out=idx_all[:, ex, j:j + 1], in0=bk[:, 128:129], in1=nloc[:, 0:1],
                                                  op=ALU.add), reads=[bbk, b_nloc], writes=[b_idx])
            P.op(P.dve, lambda e: e.scalar_tensor_tensor(out=jk[1][:], in0=iota_p[:], scalar=nloc[:, 0:1],
                                                         in1=bk[:, 256:384], op0=ALU.is_equal, op1=ALU.mult,
                                                         accum_out=gates[:, ex, j:j + 1]),
                 reads=[bbk, b_nloc, b_iota], writes=[b_jk[1], b_gates])
    pop()
    if stop_after == "B":
        return nc

    push()
    TH = max(1, CAP // 512); TN = CAP // TH
    XT = [P.sb(f"XT{i}", [128, 8, CAP], BF16) for i in range(2)]; b_XT = [B(f"XT{i}") for i in range(2)]
    actT = P.sb("actT", [128, 16, CAP], BF16); b_actT = B("actT")
    Xg = [P.sb(f"Xg{i}", [128, D], BF16) for i in range(3)]; b_Xg = [B(f"Xg{i}") for i in range(3)]
    sg = [P.sb(f"sg{i}", [128, 512], F32) for i in range(2)]; b_sg = [B(f"sg{i}") for i in range(2)]
    ysb = [P.sb(f"ysb{i}", [128, D], F32) for i in range(3)]; b_ysb = [B(f"ysb{i}") for i in range(3)]
    Wv = [B(f"wave{i}") for i in range(16)]
    B_scat = B("scat")
    cc = {"xg": 0, "sg": 0, "y": 0}

    def gather(ex):
        xt = XT[ex % 2]; bxt = b_XT[ex % 2]
        for j in range(NJ):
            sl = cc["xg"] % 3; cc["xg"] += 1
            P.dma(P.pool, lambda e: e.indirect_dma_start(out=Xg[sl][:, :], out_offset=None, in_=h16_d[:, :],
                                                         in_offset=bass.IndirectOffsetOnAxis(ap=idx_all[:, ex, j:j + 1], axis=0)),
                  b_Xg[sl], reads=[b_idx, B_h16], writes=[b_Xg[sl]])
            bk, bbk = nbank()
            bkb = bk[:].bitcast(BF16)
            for k in range(8):
                P.op(P.pe, lambda e: e.transpose(out=bkb[:, k * 128:(k + 1) * 128], in_=Xg[sl][:, k * 128:(k + 1) * 128],
                                                 identity=identb[:]), reads=[b_Xg[sl], b_id], writes=[bbk])
            P.op(P.act, lambda e: e.copy(out=xt[:, :, j * 128:(j + 1) * 128],
                                         in_=bkb[:, 0:1024].rearrange("p (k n) -> p k n", n=128)),
                 reads=[bbk], writes=[bxt])

    gather(0)
    for ex in range(16):
        xt = XT[ex % 2]; bxt = b_XT[ex % 2]
        for fb in range(4):
            n = 4 * ex + fb
            if n + 2 < 64:
                load_gu(n + 2)
            sl = n % 3
            for f4 in range(4):
                fc = 4 * fb + f4
                for th in range(TH):
                    bkG, bbG = nbank()
                    for k in range(8):
                        P.op(P.pe, lambda e: e.matmul(bkG[:, 0:TN], lhsT=Wg_r[sl][:, k, f4 * 128:(f4 + 1) * 128],
                                                      rhs=xt[:, k, th * TN:(th + 1) * TN], start=(k == 0), stop=(k == 7)),
                             reads=[b_Wg[sl], bxt], writes=[bbG])
                    bkU, bbU = nbank()
                    for k in range(8):
                        P.op(P.pe, lambda e: e.matmul(bkU[:, 0:TN], lhsT=Wu_r[sl][:, k, f4 * 128:(f4 + 1) * 128],
                                                      rhs=xt[:, k, th * TN:(th + 1) * TN], start=(k == 0), stop=(k == 7)),
                             reads=[b_Wu[sl], bxt], writes=[bbU])
                    si = cc["sg"] % 2; cc["sg"] += 1
                    P.op(P.act, lambda e: e.activation(out=sg[si][:, 0:TN], in_=bkG[:, 0:TN], func=AF.Silu),
                         reads=[bbG], writes=[b_sg[si]])
                    P.op(P.dve, lambda e: e.tensor_tensor(out=actT[:, fc, th * TN:(th + 1) * TN], in0=bkU[:, 0:TN],
                                                          in1=sg[si][:, 0:TN], op=ALU.mult),
                         reads=[bbU, b_sg[si]], writes=[b_actT])
        if ex + 1 < 16:
            gather(ex + 1)
        for j in range(NJ):
            yi = cc["y"] % 3; cc["y"] += 1
            for chh in range(2):
                bk, bbk = nbank()
                for fc in range(16):
                    P.op(P.pe, lambda e: e.matmul(bk[:, 0:512], lhsT=actT[:, fc, j * 128:(j + 1) * 128],
                                                  rhs=Wd_r[fc // 4][:, fc % 4, chh * 512:(chh + 1) * 512],
                                                  start=(fc == 0), stop=(fc == 15)),
                         reads=[b_actT, b_Wd[fc // 4]], writes=[bbk])
                P.op(P.act, lambda e: e.activation(out=ysb[yi][:, chh * 512:(chh + 1) * 512], in_=bk[:, 0:512],
                                                   func=AF.Identity, scale=gates[:, ex, j:j + 1]),
                     reads=[bbk, b_gates], writes=[b_ysb[yi]])
            prev = [Wv[ex - 1]] if ex > 0 else [B_acc]
            P.dma(P.pool, lambda e: e.indirect_dma_start(out=acc_d[:, :],
                                                         out_offset=bass.IndirectOffsetOnAxis(ap=idx_all[:, ex, j:j + 1], axis=0),
                                                         in_=ysb[yi][:, :], in_offset=None, compute_op=ALU.add),
                  b_ysb[yi], reads=[b_ysb[yi], b_idx] + prev, adds=[Wv[ex], B_scat])
        if ex + 1 < 16:
            load_d(ex + 1)
    pop()
    pop()
    if stop_after == "C":
        return nc

    push()
    ln2g = P.sb("ln2g", [128, D], F32); ln2b = P.sb("ln2b", [128, D], F32); b_ln2 = B("ln2")
    P.dma(P.sp, lambda e: e.dma_start(out=ln2g[:], in_=ln2g_d[:, :]), B("ln2g"), adds=[b_ln2])
    P.dma(P.sp, lambda e: e.dma_start(out=ln2b[:], in_=ln2b_d[:, :]), B("ln2b"), adds=[b_ln2])
    ND = 12
    at = [P.sb(f"at{i}", [128, D], F32) for i in range(ND)]; b_at2 = [B(f"at{i}") for i in range(ND)]
    ot = [P.sb(f"ot{i}", [128, D], F32) for i in range(ND)]; b_ot = [B(f"ot{i}") for i in range(ND)]
    def d_load(t):
        P.dma(P.sp, lambda e: e.dma_start(out=at[t % ND][:], in_=acc_d[t * 128:(t + 1) * 128, :]), b_at2[t % ND],
              reads=[B_acc, B_scat], writes=[b_at2[t % ND]])

    for t in range(min(ND - 1, NT)):
        d_load(t)
    def d_norm(t):
        layer_norm(at[t % ND], b_at2[t % ND], ot[t % ND], b_ot[t % ND], ln2g, ln2b, b_ln2, split=True)

    d_norm(0)
    for t in range(NT):
        o = ot[t % ND]; bo = b_ot[t % ND]
        if t + ND - 1 < NT:
            d_load(t + ND - 1)
        if t + 1 < NT:
            d_norm(t + 1)
        ln_affine(o, bo, ln2g, ln2b, b_ln2)
        P.dma(P.sp, lambda e: e.dma_start(out=out_d[t * 128:(t + 1) * 128, :], in_=o[:]), bo, reads=[bo])
    pop()
    return nc


def _perm():
    qa = np.arange(0, 512)
    qb = np.concatenate([np.concatenate([1536 + np.arange(64 * i, 64 * i + 64),
                                         1536 + np.arange(64 * (i + 4), 64 * (i + 4) + 64)]) for i in range(4)])
    ga = np.arange(2304, 3328); gb = np.arange(3328, 4352)
    ka = np.arange(512, 1024); kb = np.arange(2048, 2176)
    va = np.arange(1024, 1536); vb = np.arange(2176, 2304)
    return np.concatenate([qa, qb, ga, gb, ka, kb, va, vb])


def _na_bias(rpb, t, kts, rows):
    q = np.arange(128); r = 2 * t + q // 64; c = q % 64
    r0 = np.clip(r - 4, 0, rows - 8); cs = np.clip(c - 8, 0, 48)
    k = np.arange(128)
    out = np.full((128, 8, len(kts), 128), NEG, np.float32)
    for i, kt in enumerate(kts):
        rk = 2 * kt + k // 64; ck = k % 64
        valid = ((rk[:, None] >= r0[None, :]) & (rk[:, None] < r0[None, :] + 8)
                 & (ck[:, None] >= cs[None, :]) & (ck[:, None] < cs[None, :] + 16))
        dr = np.clip(rk[:, None] - r[None, :] + 7, 0, 14)
        dc = np.clip(ck[:, None] - c[None, :] + 15, 0, 30)
        vals = rpb[:, dr, dc]
        out[:, :, i, :] = np.where(valid[:, None, :], vals.transpose(1, 0, 2), np.float32(NEG))
    return np.ascontiguousarray(out[:, HORD]).reshape(128, 8 * len(kts) * 128)


def _kts(t, NT):
    if t < 2:
        return [0, 1, 2, 3]
    if t >= NT - 2:
        return [NT - 4, NT - 3, NT - 2, NT - 1]
    return [t - 2, t - 1, t, t + 1, t + 2]


def _wg_bias():
    k = np.arange(128)[:, None]; q = np.arange(128)[None, :]
    out = np.zeros((128, 3, 2, 4, 128), np.float32)
    for di, dt in enumerate((-1, 0, 1)):
        dist = np.abs(q - k - 128 * dt).astype(np.float32)
        for j in range(2):
            for g in range(4):
                slope = np.float32(2.0 ** (-(4 * j + g + 1)))
                out[:, di, j, g, :] = np.where(dist <= 128, -slope * dist, np.float32(NEG))
    return out.reshape(128, 24 * 128)


def _prep_shared(inp, S):
    NT = S // 128; rows = S // 64
    f = lambda a: np.ascontiguousarray(np.asarray(a), dtype=np.float32)
    perm = _perm()
    w_in = f(inp["w_in"])[0]; b_in = f(inp["b_in"])[0]
    b_p = b_in[perm]
    rpb = f(inp["rpb"])[0]
    rep = lambda v: np.ascontiguousarray(np.broadcast_to(v[None, :], (128, v.shape[0])))
    edge_tiles = [0, 1, NT - 2, NT - 1]
    sh = {
        "w_in_p": np.ascontiguousarray(w_in[:, perm]),
        "bfm": np.ascontiguousarray(b_p[:FMW].reshape(NQ, 128).T),
        "bv": rep(b_p[FMW:]),
        "nab_int": _na_bias(rpb, 2, _kts(2, NT), rows),
        "nab_edge": np.stack([_na_bias(rpb, t, _kts(t, NT), rows) for t in edge_tiles]),
        "wgb": _wg_bias(),
        "sinkb": rep(f(inp["sink"])[0]),
        "w_a": f(inp["w_branch_a"])[0], "w_b": f(inp["w_branch_b"])[0], "w_o": f(inp["w_out"])[0],
        "w_r": f(inp["w_router"])[0],
        "ln1g": rep(f(inp["ln1_g"])[0]), "ln1b": rep(f(inp["ln1_b"])[0]),
        "ln2g": rep(f(inp["ln2_g"])[0]), "ln2b": rep(f(inp["ln2_b"])[0]),
        "bd_c": (np.arange(128)[:, None] // 8 == np.arange(128)[None, :] // 8).astype(np.float32),
        "lt_c": ((np.arange(128)[:, None] // 8 == np.arange(128)[None, :] // 8)
                 & (np.arange(128)[:, None] < np.arange(128)[None, :])).astype(np.float32),
        "w_gate": f(inp["w_gate"])[0], "w_up": f(inp["w_up"])[0], "w_down": f(inp["w_down"])[0],
    }
    return sh


def _prep_core(xb):
    xb = np.ascontiguousarray(np.asarray(xb), dtype=np.float32)
    return {"xT": np.ascontiguousarray(xb.T), "x": xb}


def kernel(**inputs):
    x = np.asarray(inputs["x"])
    Bn, S, _ = x.shape
    nc = build(S)
    sh = _prep_shared(inputs, S)
    in_maps = []
    for b in range(Bn):
        m = dict(sh)
        m.update(_prep_core(x[b]))
        in_maps.append(m)
    res = run_bass_kernel_spmd(nc, in_maps, core_ids=list(range(Bn)))
    return np.stack([np.asarray(r["out"]) for r in res.results], axis=0).astype(np.float32)
```

```python
import numpy as np
import concourse.bass as bass
import concourse.mybir as mybir
from concourse.bass_utils import run_bass_kernel_spmd

F32 = mybir.dt.float32
BF16 = mybir.dt.bfloat16
I32 = mybir.dt.int32
AF = mybir.ActivationFunctionType
ALU = mybir.AluOpType
AX = mybir.AxisListType

EPOCH = 30000


class Buf:
    __slots__ = ("name", "w", "r", "dsem", "dcnt", "dcls")

    def __init__(self, name):
        self.name = name
        self.w = {}
        self.r = {}
        self.dsem = None
        self.dcnt = 0
        self.dcls = None


class Eng:
    def __init__(self, prog, name, eng):
        self.prog, self.name, self.eng = prog, name, eng
        self.n = 0
        self.sems = []
        self.seen = {}
        self.own = set()

    def next_event(self):
        ep, off = divmod(self.n, EPOCH)
        while len(self.sems) <= ep:
            s = self.prog.new_sem(f"{self.name}_e{len(self.sems)}")
            self.sems.append(s)
            self.own.add(id(s))
        self.n += 1
        return (self.sems[ep], off + 1)


class Prog:
    def __init__(self, nc):
        self.nc = nc
        self._keep = []
        self._sems = []
        self.free_sems = {}
        self.nsem = 0
        self.pe = Eng(self, "pe", nc.tensor)
        self.act = Eng(self, "act", nc.scalar)
        self.dve = Eng(self, "dve", nc.vector)
        self.pool = Eng(self, "pool", nc.gpsimd)
        self.sp = Eng(self, "sp", nc.sync)
        self.engs = [self.pe, self.act, self.dve, self.pool, self.sp]

    def new_sem(self, name):
        cm = self.nc.semaphore(name)
        s = cm.__enter__()
        self._sems.append(cm)
        self.nsem += 1
        return s

    def sb(self, name, shape, dt):
        cm = self.nc.sbuf_tensor("s_" + name, list(shape), dt)
        t = cm.__enter__()
        self._keep.append(cm)
        return t

    def ps(self, name, shape, dt):
        cm = self.nc.psum_tensor("p_" + name, list(shape), dt)
        t = cm.__enter__()
        self._keep.append(cm)
        return t

    def _deps(self, E, reads, writes):
        need = {}

        def add(ev, raw):
            sem, c = ev
            if id(sem) in E.own and not raw and E is self.pe:
                return
            k = id(sem)
            if k not in need or need[k][1] < c:
                need[k] = (sem, c)

        for b in reads:
            for ev in b.w.values():
                add(ev, True)
        for b in writes:
            for ev in b.w.values():
                add(ev, False)
            for ev in b.r.values():
                add(ev, False)
        for k, (sem, c) in need.items():
            if E.seen.get(k, 0) < c:
                E.eng.wait_ge(sem, c)
                E.seen[k] = c

    def _commit(self, ev, reads, writes):
        k = id(ev[0])
        for b in writes:
            b.w = {k: ev}
            b.r = {}
        for b in reads:
            b.r[k] = ev

    def op(self, E, fn, reads=(), writes=()):
        self._deps(E, reads, writes)
        ins = fn(E.eng)
        ev = E.next_event()
        ins.then_inc(ev[0], 1)
        self._commit(ev, reads, writes)
        return ins

    def dma(self, E, fn, semb, reads=(), writes=(), adds=()):
        self._deps(E, reads, writes)
        if semb.dsem is None:
            pool_ = self.free_sems.setdefault(E.name, [])
            if pool_:
                semb.dsem, semb.dcnt = pool_.pop()
            else:
                semb.dsem = self.new_sem("d_" + semb.name)
            semb.dcls = E.name
        assert semb.dcls == E.name, (semb.name, semb.dcls, E.name)
        ins = fn(E.eng)
        semb.dcnt += 16
        ev = (semb.dsem, semb.dcnt)
        ins.then_inc(ev[0], 16)
        self._commit(ev, reads, writes)
        for b in adds:
            b.w[id(ev[0])] = ev
        return ins

    def barrier_all(self, bufs):
        self._deps(self.sp, bufs, bufs)

    def full_barrier(self):
        evs = []
        for F in self.engs:
            if F.n > 0:
                ep, off = divmod(F.n - 1, EPOCH)
                evs.append((F.sems[ep], off + 1))
        for b in self.dbufs:
            if b.dsem is not None and b.dcnt > 0:
                evs.append((b.dsem, b.dcnt))
        for E in self.engs:
            for sem, c in evs:
                k = id(sem)
                if E.seen.get(k, 0) < c:
                    E.eng.wait_ge(sem, c)
                    E.seen[k] = c


HORD = [0, 2, 4, 6, 1, 3, 5, 7]
D = 1024
NQ = 29
FMW = NQ * 128
INW = 4352
ALPHA = 2.0 ** 0.25
NEG = -30000.0


def build(S, debug=False, stop_after=None):
    NT = S // 128
    NS = S // 512
    CAP = S // 8
    NJ = CAP // 128
    nc = bass.Bass("TRN2", target_bir_lowering=False)
    P = Prog(nc)
    P.dbufs = []

    def B(name):
        b = Buf(name)
        P.dbufs.append(b)
        return b

    def din(name, shape, dt=F32):
        return nc.dram_tensor(name, list(shape), dt, kind="ExternalInput").ap()

    def dscr(name, shape, dt):
        return nc.dram_tensor(name, list(shape), dt, kind="ExternalOutput" if debug else "Internal").ap()

    xT_d = din("xT", [D, S])
    x_d = din("x", [S, D])
    win_d = din("w_in_p", [D, INW])
    bfm_d = din("bfm", [128, NQ])
    bv_d = din("bv", [128, 640])
    nabi_d = din("nab_int", [128, 40 * 128])
    nabe_d = din("nab_edge", [4, 128, 32 * 128])
    wgb_d = din("wgb", [128, 24 * 128])
    sink_d = din("sinkb", [128, 8])
    wa_d = din("w_a", [512, D])
    wb_d = din("w_b", [512, D])
    wo_d = din("w_o", [D, D])
    wr_d = din("w_r", [D, 16])
    ln1g_d = din("ln1g", [128, D]); ln1b_d = din("ln1b", [128, D])
    ln2g_d = din("ln2g", [128, D]); ln2b_d = din("ln2b", [128, D])
    wg_d = din("w_gate", [16, D, 2048])
    wu_d = din("w_up", [16, D, 2048])
    wd_d = din("w_down", [16, 2048, D])
    bd_d = din("bd_c", [128, 128]); lt_d = din("lt_c", [128, 128])
    out_d = nc.dram_tensor("out", [S, D], F32, kind="ExternalOutput").ap()

    pfm_d = dscr("pfm", [NT, 128, FMW], BF16)
    vtm_d = dscr("vtm", [S, 640], BF16)
    h16_d = dscr("h16", [S, D], BF16)
    acc_d = dscr("acc", [S, D], F32)
    affT_d = dscr("affT", [16, S], F32)
    C_d = dscr("Ccum", [16, S], F32)

    banks = [P.ps(f"bank{i}", [128, 512], F32) for i in range(8)]
    bbank = [B(f"bank{i}") for i in range(8)]
    rot = {"i": 0, "n": 8}

    busy = [False] * 8
    rot["strict"] = False

    def nbank():
        for _ in range(rot["n"]):
            i = rot["i"] % rot["n"]
            rot["i"] += 1
            if not busy[i]:
                break
        else:
            raise RuntimeError("no free PSUM bank")
        if rot["strict"]:
            busy[i] = True
        return banks[i], bbank[i]

    def rel(bb):
        busy[bbank.index(bb)] = False

    identb = P.sb("identb", [128, 128], BF16)
    identf = P.sb("identf", [128, 128], F32)
    b_id = B("ident")
    P.op(P.pool, lambda e: e.memset(identf[:], 0.0), writes=[b_id])
    P.op(P.pool, lambda e: e.affine_select(out=identf[:], in_=identf[:], pattern=[[-1, 128]],
                                           compare_op=ALU.not_equal, fill=1.0, base=0,
                                           channel_multiplier=1), reads=[b_id], writes=[b_id])
    P.op(P.dve, lambda e: e.tensor_copy(out=identb[:], in_=identf[:]), reads=[b_id], writes=[b_id])

    LNS = 12
    stats_l = [P.sb(f"stats{i}", [128, 2, 6], F32) for i in range(LNS)]; b_stats_l = [B(f"stats{i}") for i in range(LNS)]
    mv_l = [P.sb(f"mv{i}", [128, 2], F32) for i in range(LNS)]; b_mv_l = [B(f"mv{i}") for i in range(LNS)]
    lnv_l = [P.sb(f"lnv{i}", [128, 4], F32) for i in range(LNS)]; b_lnv_l = [B(f"lnv{i}") for i in range(LNS)]
    lnc = {"i": 0}
    mhalf = P.sb("mhalf", [128, 1], F32); b_mhalf = B("mhalf")
    P.op(P.pool, lambda e: e.memset(mhalf[:], -0.5), writes=[b_mhalf])
    mone = P.sb("mone", [128, 1], F32)
    P.op(P.pool, lambda e: e.memset(mone[:], -1.0), writes=[b_mhalf])
    idx_all = P.sb("idx_all", [128, 16, NJ], I32); gates = P.sb("gates", [128, 16, NJ], F32)
    b_idx = B("idx_all"); b_gates = B("gates")
    B_h16 = B("h16d"); B_acc = B("accd"); B_affT = B("affTd"); B_pfm = B("pfm"); B_vtm = B("vtm")

    scopes = []

    def push():
        scopes.append((len(P._keep), len(P.dbufs)))

    def pop():
        P.full_barrier()
        n, nb = scopes.pop()
        while len(P._keep) > n:
            P._keep.pop().__exit__(None, None, None)
        for b in P.dbufs[nb:]:
            if b.dsem is not None:
                P.free_sems.setdefault(b.dcls, []).append((b.dsem, b.dcnt))
                b.dsem = None
        del P.dbufs[nb:]

    push()
    W = P.sb("W_in", [128, 8, INW], BF16); b_W = B("W_in")
    win_v = win_d.rearrange("(k p) n -> p k n", p=128)
    xs = [P.sb(f"xs{i}", [128, 8, 512], BF16) for i in range(2)]
    b_xs = [B(f"xs{i}") for i in range(2)]
    xT_v = xT_d.rearrange("(k p) n -> p k n", p=128)

    def a1_load(s):
        P.dma(P.pool, lambda e: e.dma_start(out=xs[s % 2][:], in_=xT_v[:, :, s * 512:(s + 1) * 512]),
              b_xs[s % 2], writes=[b_xs[s % 2]])

    b_Wb_ = []
    for bi in range((INW + 511) // 512):
        c0, c1 = bi * 512, min(INW, bi * 512 + 512)
        bb = B(f"Wld{bi}")
        P.dma(P.pool, lambda e: e.dma_start(out=W[:, :, c0:c1], in_=win_v[:, :, c0:c1]), bb, writes=[bb])
        b_Wb_.append(bb)
        if bi == 0:
            a1_load(0)

    def wdeps(c0, c1):
        return [b_Wb_[i] for i in range(c0 // 512, (c1 - 1) // 512 + 1)]
    bfm = P.sb("bfm", [128, NQ], F32); bq8 = P.sb("bq8", [128, 8], F32); b_bfm = B("bfm")
    bv = P.sb("bv", [128, 640], F32); b_bv = B("bv")
    P.dma(P.sp, lambda e: e.dma_start(out=bfm[:], in_=bfm_d[:, :]), b_bfm, writes=[b_bfm])
    P.dma(P.sp, lambda e: e.dma_start(out=bv[:], in_=bv_d[:, :]), b_bv, writes=[b_bv])
    b_bq8 = B("bq8")
    P.op(P.dve, lambda e: e.tensor_scalar(out=bq8[:], in0=bfm[:, 0:8], scalar1=0.125, scalar2=None,
                                          op0=ALU.mult), reads=[b_bfm], writes=[b_bq8])
    stage = [P.sb(f"stage{i}", [128, 4, NQ, 128], BF16) for i in range(2)]
    b_stage = [B(f"stage{i}") for i in range(2)]
    vst = [P.sb(f"vst{i}", [128, 4, 640], BF16) for i in range(2)]
    b_vst = [B(f"vst{i}") for i in range(2)]
    for s in range(NS):
        if s + 1 < NS:
            a1_load(s + 1)
        xb, bx = xs[s % 2], b_xs[s % 2]
        st, bst = stage[s % 2], b_stage[s % 2]
        vs, bvs = vst[s % 2], b_vst[s % 2]
        for c in range(NQ):
            bk, bbk = nbank()
            for k in range(8):
                P.op(P.pe, lambda e: e.matmul(bk[:, 0:512], lhsT=W[:, k, c * 128:(c + 1) * 128],
                                              rhs=xb[:, k, :], start=(k == 0), stop=(k == 7)),
                     reads=wdeps(c * 128, c * 128 + 128) + [bx], writes=[bbk])
            src = bk[:, 0:512].rearrange("p (t n) -> p t n", n=128)
            if c < 8:
                P.op(P.act, lambda e: e.activation(out=st[:, :, c, :], in_=src, func=AF.Identity,
                                                   bias=bq8[:, c:c + 1], scale=0.125),
                     reads=[bbk, b_bq8], writes=[bst])
            elif c < 24:
                P.op(P.act, lambda e: e.activation(out=st[:, :, c, :], in_=src, func=AF.Sigmoid,
                                                   bias=bfm[:, c:c + 1], scale=1.0),
                     reads=[bbk, b_bfm], writes=[bst])
            else:
                P.op(P.act, lambda e: e.activation(out=st[:, :, c, :], in_=src, func=AF.Identity,
                                                   bias=bfm[:, c:c + 1], scale=1.0),
                     reads=[bbk, b_bfm], writes=[bst])
        for i in range(4):
            bk, bbk = nbank()
            for k in range(8):
                P.op(P.pe, lambda e: e.matmul(bk[:, 0:512], lhsT=xb[:, k, i * 128:(i + 1) * 128],
                                              rhs=W[:, k, FMW:FMW + 512], start=(k == 0), stop=(k == 7)),
                     reads=wdeps(FMW, FMW + 512) + [bx], writes=[bbk])
            P.op(P.dve, lambda e: e.tensor_tensor(out=vs[:, i, 0:512], in0=bk[:, 0:512], in1=bv[:, 0:512],
                                                  op=ALU.add), reads=[bbk, b_bv], writes=[bvs])
            bk, bbk = nbank()
            for k in range(8):
                P.op(P.pe, lambda e: e.matmul(bk[:, 0:128], lhsT=xb[:, k, i * 128:(i + 1) * 128],
                                              rhs=W[:, k, FMW + 512:INW], start=(k == 0), stop=(k == 7)),
                     reads=wdeps(FMW + 512, INW) + [bx], writes=[bbk])
            P.op(P.dve, lambda e: e.tensor_tensor(out=vs[:, i, 512:640], in0=bk[:, 0:128], in1=bv[:, 512:640],
                                                  op=ALU.add), reads=[bbk, b_bv], writes=[bvs])
        P.dma(P.sp, lambda e: e.dma_start(
            out=pfm_d[4 * s:4 * s + 4].rearrange("t p f -> p t f"),
            in_=st[:].rearrange("p t c n -> p t (c n)")), bst, reads=[bst], adds=[B_pfm])
        P.dma(P.sp, lambda e: e.dma_start(
            out=vtm_d.rearrange("(t p) f -> p t f", p=128)[:, 4 * s:4 * s + 4, :],
            in_=vs[:]), bvs, reads=[bvs], adds=[B_vtm])
    pop()
    if stop_after == "A1":
        return nc

    push()
    rot["n"] = 8; rot["i"] = 0; rot["strict"] = True
    Wa = P.sb("Wa", [128, 4, D], BF16); Wb = P.sb("Wb", [128, 4, D], BF16); Wo = P.sb("Wo", [128, 8, D], BF16)
    Wr = P.sb("Wr", [128, 8, 16], F32)
    b_Wa = B("Wa"); b_Wb = B("Wb"); b_Wo = B("Wo"); b_Wr = B("Wr")
    def load_merge_w():
        P.dma(P.pool, lambda e: e.dma_start(out=Wa[:], in_=wa_d.rearrange("(k p) n -> p k n", p=128)), b_Wa, writes=[b_Wa])
        P.dma(P.pool, lambda e: e.dma_start(out=Wb[:], in_=wb_d.rearrange("(k p) n -> p k n", p=128)), b_Wb, writes=[b_Wb])
        P.dma(P.pool, lambda e: e.dma_start(out=Wo[:], in_=wo_d.rearrange("(k p) n -> p k n", p=128)), b_Wo, writes=[b_Wo])
    P.dma(P.sp, lambda e: e.dma_start(out=Wr[:], in_=wr_d.rearrange("(k p) n -> p k n", p=128)), b_Wr, writes=[b_Wr])
    nabi = P.sb("nabi", [128, 40 * 128], BF16); b_nabi = B("nabi")
    nabe = P.sb("nabe", [128, 32 * 128], BF16); b_nabe = B("nabe")
    wgb = P.sb("wgb", [128, 24 * 128], BF16); b_wgb = B("wgb")
    for c0 in range(0, 40 * 128, 2048):
        c1 = min(c0 + 2048, 40 * 128)
        P.dma(P.pool, lambda e: e.dma_start(out=nabi[:, c0:c1], in_=nabi_d[:, c0:c1]), B(f"nabi{c0}"), adds=[b_nabi])
    for c0 in range(0, 24 * 128, 1536):
        P.dma(P.pool, lambda e: e.dma_start(out=wgb[:, c0:c0 + 1536], in_=wgb_d[:, c0:c0 + 1536]), B(f"wgb{c0}"), adds=[b_wgb])
    ln1g = P.sb("ln1g", [128, D], F32); ln1b = P.sb("ln1b", [128, D], F32); b_ln1 = B("ln1")
    P.dma(P.sp, lambda e: e.dma_start(out=ln1g[:], in_=ln1g_d[:, :]), B("ln1g"), adds=[b_ln1])
    P.dma(P.sp, lambda e: e.dma_start(out=ln1b[:], in_=ln1b_d[:, :]), B("ln1b"), adds=[b_ln1])
    esink = P.sb("esink", [128, 8, 1], F32); b_esink = B("esink")
    P.dma(P.sp, lambda e: e.dma_start(out=esink[:].rearrange("p h o -> p (h o)"), in_=sink_d[:, :]), b_esink, writes=[b_esink])
    P.op(P.act, lambda e: e.activation(out=esink[:], in_=esink[:], func=AF.Exp), reads=[b_esink], writes=[b_esink])

    Q2 = [P.sb(f"Q2_{i}", [128, 8, 128], BF16) for i in range(2)]; b_Q2 = [B(f"Q2_{i}") for i in range(2)]
    G4 = [P.sb(f"G4_{i}", [128, 16, 128], BF16) for i in range(4)]; b_G4 = [B(f"G4_{i}") for i in range(4)]
    Kr = [P.sb(f"Kr{i}", [128, 5, 128], BF16) for i in range(8)]; b_K = [B(f"Kr{i}") for i in range(8)]
    Vr = [P.sb(f"Vr{i}", [128, 10, 65], BF16) for i in range(8)]; b_V = [B(f"Vr{i}") for i in range(8)]
    for i in range(8):
        P.op(P.pool, lambda e: e.memset(Vr[i][:, :, 64:65], 1.0), writes=[b_V[i]])
    xtok = [P.sb(f"xtok{i}", [128, D], F32) for i in range(2)]; b_xtok = [B(f"xtok{i}") for i in range(2)]
    sc = [P.sb(f"sc{i}", [128, 512], F32) for i in range(3)]; b_sc = [B(f"sc{i}") for i in range(3)]
    PTa2 = [P.sb(f"PTa{i}", [128, 40, 128], BF16) for i in range(2)]; b_PTa2 = [B(f"PTa{i}") for i in range(2)]
    PTb2 = [P.sb(f"PTb{i}", [128, 3, 2, 4, 128], BF16) for i in range(2)]; b_PTb2 = [B(f"PTb{i}") for i in range(2)]
    rden = P.sb("rden", [128, 16, 1], F32); b_rden = B("rden")
    ya = P.sb("ya", [128, 8, 64], BF16); yb = P.sb("yb", [128, 8, 64], BF16); b_ya = B("ya"); b_yb = B("yb")
    yT2 = [P.sb(f"yT{i}", [128, 8, 128], BF16) for i in range(2)]; b_yT2 = [B(f"yT{i}") for i in range(2)]
    t1 = P.sb("t1", [128, 512], F32); t2 = P.sb("t2", [128, 512], F32); b_t1 = B("t1"); b_t2 = B("t2")
    mixT2 = [P.sb(f"mixT{i}", [128, 8, 128], BF16) for i in range(2)]; b_mixT2 = [B(f"mixT{i}") for i in range(2)]
    rr = [P.sb(f"rr{i}", [128, D], F32) for i in range(2)]; b_rr = [B(f"rr{i}") for i in range(2)]
    hh = [P.sb(f"hh{i}", [128, D], F32) for i in range(2)]; b_hh = [B(f"hh{i}") for i in range(2)]
    h16 = [P.sb(f"h16_{i}", [128, D], BF16) for i in range(2)]; b_h16s = [B(f"h16_{i}") for i in range(2)]
    hT = P.sb("hT", [128, 8, 128], F32); b_hT = B("hT")
    sm = P.sb("sm", [128, 4], F32); b_sm = B("sm")
    e16 = P.sb("e16", [128, 16], F32); aff = P.sb("aff", [128, 16], F32); b_e16 = B("e16"); b_aff = B("aff")
    affTs = [P.sb(f"affTs{i}", [16, 128], F32) for i in range(2)]; b_affTs = [B(f"affTs{i}") for i in range(2)]
    cnt = {"sc": 0}
    edge_tiles = [0, 1, NT - 2, NT - 1]

    def load_kv(kt):
        sl = kt % 8
        P.dma(P.sp, lambda e: e.dma_start(out=Kr[sl][:].rearrange("p c n -> p (c n)"), in_=pfm_d[kt, :, 3072:FMW]),
              b_K[sl], reads=[B_pfm], writes=[b_K[sl]])
        P.dma(P.sp, lambda e: e.dma_start(out=Vr[sl][:, :, 0:64],
                                          in_=vtm_d[kt * 128:(kt + 1) * 128, :].rearrange("p (h d) -> p h d", d=64)),
              b_V[sl], reads=[B_vtm], writes=[b_V[sl]])

    def load_q(t):
        sl = t % 2
        P.dma(P.sp, lambda e: e.dma_start(out=Q2[sl][:].rearrange("p c n -> p (c n)"), in_=pfm_d[t, :, 0:1024]),
              b_Q2[sl], reads=[B_pfm], writes=[b_Q2[sl]])
        g = t % 4
        P.dma(P.sp, lambda e: e.dma_start(out=G4[g][:].rearrange("p c n -> p (c n)"), in_=pfm_d[t, :, 1024:3072]),
              b_G4[g], reads=[B_pfm], writes=[b_G4[g]])

    def load_x(t):
        sl = t % 2
        P.dma(P.sp, lambda e: e.dma_start(out=xtok[sl][:], in_=x_d[t * 128:(t + 1) * 128, :]),
              b_xtok[sl], writes=[b_xtok[sl]])

    def stage1(t):
        kts = _kts(t, NT); nk = len(kts)
        q = Q2[t % 2]; bq = b_Q2[t % 2]
        PTa = PTa2[t % 2]; b_PTa = b_PTa2[t % 2]; PTb = PTb2[t % 2]; b_PTb = b_PTb2[t % 2]
        if nk == 5:
            nab, bnab = nabi, b_nabi
        else:
            ei = edge_tiles.index(t)
            for c0 in range(0, 32 * 128, 2048):
                P.dma(P.pool, lambda e: e.dma_start(out=nabe[:, c0:c0 + 2048], in_=nabe_d[ei, :, c0:c0 + 2048]),
                      B(f"nabe{t}_{c0}"), writes=[b_nabe] if c0 == 0 else [], adds=[b_nabe] if c0 else [])
            nab, bnab = nabe, b_nabe
        for uu in range(2 * nk):
            u = (uu // 2) + nk * (uu % 2)
            bk, bbk = nbank()
            for jj in range(4):
                hp, i = divmod(4 * u + jj, nk); h = HORD[hp]; kt = kts[i]; ch, half = divmod(h, 2)
                sl = slice(64 * half, 64 * half + 64)
                P.op(P.pe, lambda e: e.matmul(bk[:, jj * 128:(jj + 1) * 128], lhsT=Kr[kt % 8][sl, ch, :],
                                              rhs=q[sl, ch, :], start=True, stop=True),
                     reads=[b_K[kt % 8], bq], writes=[bbk])
            si = cnt["sc"] % 3; cnt["sc"] += 1
            P.op(P.dve, lambda e: e.tensor_tensor(out=sc[si][:], in0=bk[:, 0:512], in1=nab[:, u * 512:(u + 1) * 512],
                                                  op=ALU.add), reads=[bbk, bnab], writes=[b_sc[si]])
            rel(bbk)
            P.op(P.act, lambda e: e.activation(out=PTa[:, 4 * u:4 * u + 4, :].rearrange("p b n -> p (b n)"),
                                               in_=sc[si][:], func=AF.Exp), reads=[b_sc[si]], writes=[b_PTa])
            yield
        dts = [dt for dt in (-1, 0, 1) if 0 <= t + dt < NT]
        for j in range(2):
            sl = slice(64 * j, 64 * j + 64)
            for dt in dts:
                di = dt + 1; kt = t + dt
                bk, bbk = nbank()
                P.op(P.pe, lambda e: e.matmul(bk[:, 0:512], lhsT=Kr[kt % 8][sl, 4, :],
                                              rhs=q[sl, 4:8, :].rearrange("p c n -> p (c n)"), start=True, stop=True),
                     reads=[b_K[kt % 8], bq], writes=[bbk])
                si = cnt["sc"] % 3; cnt["sc"] += 1
                o = (di * 2 + j) * 512
                P.op(P.dve, lambda e: e.tensor_tensor(out=sc[si][:], in0=bk[:, 0:512], in1=wgb[:, o:o + 512],
                                                      op=ALU.add), reads=[bbk, b_wgb], writes=[b_sc[si]])
                rel(bbk)
                P.op(P.act, lambda e: e.activation(out=PTb[:, di, j].rearrange("p g n -> p (g n)"),
                                                   in_=sc[si][:], func=AF.Exp), reads=[b_sc[si]], writes=[b_PTb])
                yield

    def s2_pv(t):
        kts = _kts(t, NT); nk = len(kts)
        PTa = PTa2[t % 2]; b_PTa = b_PTa2[t % 2]; PTb = PTb2[t % 2]; b_PTb = b_PTb2[t % 2]
        dts = [dt for dt in (-1, 0, 1) if 0 <= t + dt < NT]
        for hb in range(2):
            pv, bpv = nbank()
            for h in range(4 * hb, 4 * hb + 4):
                col = (h % 4) * 65
                for i, kt in enumerate(kts):
                    P.op(P.pe, lambda e: e.matmul(pv[:, col:col + 65], lhsT=PTa[:, HORD.index(h) * nk + i, :],
                                                  rhs=Vr[kt % 8][:, h, :], start=(i == 0), stop=(i == nk - 1)),
                         reads=[b_PTa, b_V[kt % 8]], writes=[bpv])
                yield
            pvv = pv[:, 0:260].rearrange("p (h d) -> p h d", d=65)
            P.op(P.dve, lambda e: e.reciprocal(out=rden[:, 4 * hb:4 * hb + 4, :], in_=pvv[:, :, 64:65]),
                 reads=[bpv], writes=[b_rden])
            P.op(P.dve, lambda e: e.tensor_tensor(out=ya[:, 4 * hb:4 * hb + 4, :], in0=pvv[:, :, 0:64],
                                                  in1=rden[:, 4 * hb:4 * hb + 4, :].to_broadcast([128, 4, 64]),
                                                  op=ALU.mult), reads=[bpv, b_rden], writes=[b_ya])
            rel(bpv)
        for hb in range(2):
            pv, bpv = nbank()
            for hd in range(4 * hb, 4 * hb + 4):
                j, g = divmod(hd, 4); col = (hd % 4) * 65
                for ii, dt in enumerate(dts):
                    kt = t + dt
                    P.op(P.pe, lambda e: e.matmul(pv[:, col:col + 65], lhsT=PTb[:, dt + 1, j, g, :],
                                                  rhs=Vr[kt % 8][:, 8 + j, :], start=(ii == 0), stop=(ii == len(dts) - 1)),
                         reads=[b_PTb, b_V[kt % 8]], writes=[bpv])
                yield
            pvv = pv[:, 0:260].rearrange("p (h d) -> p h d", d=65)
            rs = rden[:, 8 + 4 * hb:12 + 4 * hb, :]
            P.op(P.dve, lambda e: e.tensor_tensor(out=rs, in0=pvv[:, :, 64:65], in1=esink[:, 4 * hb:4 * hb + 4, :],
                                                  op=ALU.add), reads=[bpv, b_esink], writes=[b_rden])
            P.op(P.dve, lambda e: e.reciprocal(out=rs, in_=rs), reads=[b_rden], writes=[b_rden])
            P.op(P.dve, lambda e: e.tensor_tensor(out=yb[:, 4 * hb:4 * hb + 4, :], in0=pvv[:, :, 0:64],
                                                  in1=rs.to_broadcast([128, 4, 64]), op=ALU.mult),
                 reads=[bpv, b_rden], writes=[b_yb])
            rel(bpv)

    def s2_tr(t):
        yT = yT2[t % 2]; b_yT = b_yT2[t % 2]
        bk, bbk = nbank()
        bkb = bk[:].bitcast(BF16)
        yaf = ya[:].rearrange("p h d -> p (h d)"); ybf = yb[:].rearrange("p h d -> p (h d)")
        for c in range(4):
            P.op(P.pe, lambda e: e.transpose(out=bkb[:, c * 128:(c + 1) * 128], in_=yaf[:, c * 128:(c + 1) * 128],
                                             identity=identb[:]), reads=[b_ya, b_id], writes=[bbk])
        for c in range(4):
            P.op(P.pe, lambda e: e.transpose(out=bkb[:, 512 + c * 128:512 + (c + 1) * 128],
                                             in_=ybf[:, c * 128:(c + 1) * 128], identity=identb[:]),
                 reads=[b_yb, b_id], writes=[bbk])
        P.op(P.act, lambda e: e.copy(out=yT[:].rearrange("p c n -> p (c n)"), in_=bkb[:, 0:1024]),
             reads=[bbk], writes=[b_yT])
        rel(bbk)
        yield

    def s2_br(t):
        yT = yT2[t % 2]; b_yT = b_yT2[t % 2]
        mixT = mixT2[t % 2]; b_mixT = b_mixT2[t % 2]
        g4 = G4[t % 4]; bq = b_G4[t % 4]
        for half in range(2):
            bkX, bbX = nbank()
            for c in range(4):
                for k in range(4):
                    P.op(P.pe, lambda e: e.matmul(bkX[:, c * 128:(c + 1) * 128],
                                                  lhsT=Wa[:, k, (4 * half + c) * 128:(4 * half + c + 1) * 128],
                                                  rhs=yT[:, k, :], start=(k == 0), stop=(k == 3)),
                         reads=[b_Wa, b_yT], writes=[bbX])
            yield
            bkY, bbY = nbank()
            for c in range(4):
                for k in range(4):
                    P.op(P.pe, lambda e: e.matmul(bkY[:, c * 128:(c + 1) * 128],
                                                  lhsT=Wb[:, k, (4 * half + c) * 128:(4 * half + c + 1) * 128],
                                                  rhs=yT[:, 4 + k, :], start=(k == 0), stop=(k == 3)),
                         reads=[b_Wb, b_yT], writes=[bbY])
            yield
            P.op(P.dve, lambda e: e.tensor_tensor(out=t1[:], in0=bkX[:, 0:512],
                                                  in1=g4[:, 4 * half:4 * half + 4, :].rearrange("p c n -> p (c n)"),
                                                  op=ALU.mult), reads=[bbX, bq], writes=[b_t1])
            rel(bbX)
            P.op(P.dve, lambda e: e.tensor_tensor(out=t2[:], in0=bkY[:, 0:512],
                                                  in1=g4[:, 8 + 4 * half:12 + 4 * half, :].rearrange("p c n -> p (c n)"),
                                                  op=ALU.mult), reads=[bbY, bq], writes=[b_t2])
            rel(bbY)
            P.op(P.pool, lambda e: e.tensor_tensor(out=mixT[:, 4 * half:4 * half + 4, :].rearrange("p c n -> p (c n)"),
                                                   in0=t1[:], in1=t2[:], op=ALU.add),
                 reads=[b_t1, b_t2], writes=[b_mixT])

    def s2_mix_b(t):
        mixT = mixT2[t % 2]; b_mixT = b_mixT2[t % 2]
        r = rr[t % 2]; br = b_rr[t % 2]
        xt = xtok[t % 2]; bxt = b_xtok[t % 2]
        for chh in range(2):
            bk, bbk = nbank()
            for k in range(8):
                P.op(P.pe, lambda e: e.matmul(bk[:, 0:512], lhsT=mixT[:, k, :], rhs=Wo[:, k, chh * 512:(chh + 1) * 512],
                                              start=(k == 0), stop=(k == 7)), reads=[b_mixT, b_Wo], writes=[bbk])
            P.op(P.dve, lambda e: e.scalar_tensor_tensor(out=r[:, chh * 512:(chh + 1) * 512],
                                                         in0=xt[:, chh * 512:(chh + 1) * 512], scalar=ALPHA,
                                                         in1=bk[:, 0:512], op0=ALU.mult, op1=ALU.add),
                 reads=[bbk, bxt], writes=[br])
            rel(bbk)
            yield

    def s2_ln(t):
        r = rr[t % 2]; br = b_rr[t % 2]; h = hh[t % 2]; bh = b_hh[t % 2]
        layer_norm(r, br, h, bh, ln1g, ln1b, b_ln1, g_on_pool=True)
        h6 = h16[t % 2]; bh6 = b_h16s[t % 2]
        P.op(P.act, lambda e: e.copy(out=h6[:], in_=h[:]), reads=[bh], writes=[bh6])
        P.op(P.act, lambda e: e.activation(out=r[:], in_=h[:], func=AF.Identity, scale=ALPHA), reads=[bh], writes=[br])
        P.dma(P.sp, lambda e: e.dma_start(out=h16_d[t * 128:(t + 1) * 128, :], in_=h6[:]), bh6, reads=[bh6], adds=[B_h16])
        P.dma(P.sp, lambda e: e.dma_start(out=acc_d[t * 128:(t + 1) * 128, :], in_=r[:]), br, reads=[br], adds=[B_acc])

    def s2_ln_b(t):
        h = hh[t % 2]; bh = b_hh[t % 2]
        for kb in range(2):
            bk, bbk = nbank()
            for k4 in range(4):
                k = 4 * kb + k4
                P.op(P.pe, lambda e: e.transpose(out=bk[:, k4 * 128:(k4 + 1) * 128], in_=h[:, k * 128:(k + 1) * 128],
                                                 identity=identf[:]), reads=[bh, b_id], writes=[bbk])
            P.op(P.act, lambda e: e.copy(out=hT[:, 4 * kb:4 * kb + 4, :].rearrange("p c n -> p (c n)"), in_=bk[:, 0:512]),
                 reads=[bbk], writes=[b_hT])
            rel(bbk)
            yield
        bk, bbk = nbank()
        for k in range(8):
            P.op(P.pe, lambda e: e.matmul(bk[:, 0:16], lhsT=hT[:, k, :], rhs=Wr[:, k, :], start=(k == 0), stop=(k == 7)),
                 reads=[b_hT, b_Wr], writes=[bbk])
        yield
        P.op(P.act, lambda e: e.activation(out=e16[:], in_=bk[:, 0:16], func=AF.Exp, accum_out=sm[:, 2:3]),
             reads=[bbk], writes=[b_e16, b_sm])
        rel(bbk)
        P.op(P.pool, lambda e: e.tensor_tensor(out=sm[:, 3:4], in0=sm[:, 2:3], in1=mone[:], op=ALU.pow),
             reads=[b_sm, b_mhalf], writes=[b_sm])
        P.op(P.pool, lambda e: e.tensor_scalar(out=aff[:], in0=e16[:], scalar1=sm[:, 3:4], scalar2=1.0,
                                               op0=ALU.mult, op1=ALU.mult), reads=[b_e16, b_sm], writes=[b_aff])
        bk, bbk = nbank()
        P.op(P.pe, lambda e: e.transpose(out=bk[0:16, 0:128], in_=aff[:, 0:16], identity=identf[:]),
             reads=[b_aff, b_id], writes=[bbk])
        ats = affTs[t % 2]; bats = b_affTs[t % 2]
        P.op(P.act, lambda e: e.copy(out=ats[:], in_=bk[0:16, 0:128]), reads=[bbk], writes=[bats])
        rel(bbk)
        P.dma(P.sp, lambda e: e.dma_start(out=affT_d[:, t * 128:(t + 1) * 128], in_=ats[:]), bats, reads=[bats], adds=[B_affT])

    def layer_norm(r, br, h, bh, g, b, bgb, g_on_pool=False, split=False):
        li = lnc["i"] % LNS; lnc["i"] += 1
        stats, mv, lnv = stats_l[li], mv_l[li], lnv_l[li]
        b_stats, b_mv, b_lnv = b_stats_l[li], b_mv_l[li], b_lnv_l[li]
        for c in range(2):
            P.op(P.dve, lambda e: e.bn_stats(out=stats[:, c, :], in_=r[:, c * 512:(c + 1) * 512]), reads=[br], writes=[b_stats])
        P.op(P.dve, lambda e: e.bn_aggr(out=mv[:], in_=stats[:].rearrange("p a b -> p (a b)")), reads=[b_stats], writes=[b_mv])
        P.op(P.dve, lambda e: e.tensor_scalar(out=lnv[:, 0:1], in0=mv[:, 1:2], scalar1=1e-5, scalar2=None, op0=ALU.add),
             reads=[b_mv], writes=[b_lnv])
        P.op(P.pool, lambda e: e.tensor_tensor(out=lnv[:, 1:2], in0=lnv[:, 0:1], in1=mhalf[:], op=ALU.pow),
             reads=[b_lnv, b_mhalf], writes=[b_lnv])
        P.op(P.pool, lambda e: e.tensor_scalar(out=lnv[:, 2:3], in0=mv[:, 0:1], scalar1=-1.0, scalar2=lnv[:, 1:2],
                                               op0=ALU.mult, op1=ALU.mult), reads=[b_mv, b_lnv], writes=[b_lnv])
        P.op(P.act, lambda e: e.activation(out=h[:], in_=r[:], func=AF.Identity, bias=lnv[:, 2:3], scale=lnv[:, 1:2]),
             reads=[br, b_lnv], writes=[bh])
        if not split:
            ln_affine(h, bh, g, b, bgb, g_on_pool)

    def ln_affine(h, bh, g, b, bgb, g_on_pool=False):
        ge_ = P.pool if g_on_pool else P.dve
        P.op(ge_, lambda e: e.tensor_tensor(out=h[:], in0=h[:], in1=g[:], op=ALU.mult), reads=[bh, bgb], writes=[bh])
        P.op(P.pool, lambda e: e.tensor_tensor(out=h[:], in0=h[:], in1=b[:], op=ALU.add), reads=[bh, bgb], writes=[bh])

    import itertools

    def drive(gA, gBs):
        gBs = list(gBs)
        aA = gA is not None
        while aA or gBs:
            if aA:
                try:
                    next(gA)
                except StopIteration:
                    aA = False
            for g in list(gBs):
                try:
                    next(g)
                except StopIteration:
                    gBs.remove(g)

    for kt in range(min(5, NT)):
        load_kv(kt)
    load_q(0)
    drive(stage1(0), [])
    load_merge_w()
    for t in range(NT + 4):
        if t + 5 < NT:
            load_kv(t + 5)
        if t + 1 < NT:
            load_q(t + 1)
        if 0 <= t - 1 < NT:
            load_x(t - 1)
        if 0 <= t - 3 < NT:
            s2_ln(t - 3)
        gens = []
        if t < NT:
            gens.append(itertools.chain(s2_pv(t), s2_tr(t)))
        if 0 <= t - 1 < NT:
            gens.append(s2_br(t - 1))
        if 0 <= t - 2 < NT:
            gens.append(s2_mix_b(t - 2))
        if 0 <= t - 4 < NT:
            gens.append(s2_ln_b(t - 4))
        drive(stage1(t + 1) if t + 1 < NT else None, gens)
    pop()
    rot["n"] = 8; rot["strict"] = False
    if stop_after == "A2":
        return nc

    push()
    Wg_r = [P.sb(f"Wg{i}", [128, 8, 512], BF16) for i in range(3)]; b_Wg = [B(f"Wg{i}") for i in range(3)]
    Wu_r = [P.sb(f"Wu{i}", [128, 8, 512], BF16) for i in range(3)]; b_Wu = [B(f"Wu{i}") for i in range(3)]
    Wd_r = [P.sb(f"Wd{i}", [128, 4, D], BF16) for i in range(4)]; b_Wd = [B(f"Wd{i}") for i in range(4)]
    def load_gu(n):
        ex, fb = divmod(n, 4); sl = n % 3
        P.dma(P.pool, lambda e: e.dma_start(out=Wg_r[sl][:], in_=wg_d[ex].rearrange("(k p) n -> p k n", p=128)[:, :, fb * 512:(fb + 1) * 512]),
              b_Wg[sl], writes=[b_Wg[sl]])
        P.dma(P.pool, lambda e: e.dma_start(out=Wu_r[sl][:], in_=wu_d[ex].rearrange("(k p) n -> p k n", p=128)[:, :, fb * 512:(fb + 1) * 512]),
              b_Wu[sl], writes=[b_Wu[sl]])

    def load_d(ex):
        for fb in range(4):
            P.dma(P.pool, lambda e: e.dma_start(out=Wd_r[fb][:], in_=wd_d[ex, fb * 512:(fb + 1) * 512, :].rearrange("(c p) n -> p c n", p=128)),
                  b_Wd[fb], writes=[b_Wd[fb]])

    load_gu(0); load_gu(1); load_d(0)

    push()
    G8 = 8; SG = S // G8
    affS = P.sb("affS", [128, SG], F32); b_affS = B("affS")
    P.dma(P.sp, lambda e: e.dma_start(out=affS[:], in_=affT_d.rearrange("e (g n) -> (e g) n", g=G8)), b_affS,
          reads=[B_affT], writes=[b_affS])
    junk = P.sb("junk", [128, SG], F32); b_junk = B("junk")
    onesS = P.sb("onesS", [128, SG], F32); b_ones = B("onesS")
    Cs = P.sb("Cs", [128, SG], F32); b_Cs = B("Cs")
    bis = P.sb("bis", [128, 8], F32); b_bis = B("bis")
    BD = P.sb("BD", [128, 128], F32); LT = P.sb("LT", [128, 128], F32); b_BD = B("BD")
    P.dma(P.sp, lambda e: e.dma_start(out=BD[:], in_=bd_d[:, :]), B("BDl"), adds=[b_BD])
    P.dma(P.sp, lambda e: e.dma_start(out=LT[:], in_=lt_d[:, :]), B("LTl"), adds=[b_BD])
    P.op(P.pool, lambda e: e.memset(onesS[:], 1.0), writes=[b_ones])
    P.op(P.dve, lambda e: e.memset(bis[:, 0:1], 0.0), writes=[b_bis])
    P.op(P.dve, lambda e: e.memset(bis[:, 1:2], 1.0), reads=[b_bis], writes=[b_bis])
    lo, hi, mid, cn, ge, dd = [bis[:, i:i + 1] for i in range(6)]
    for it in range(32):
        P.op(P.dve, lambda e: e.tensor_scalar(out=mid, in0=lo, scalar1=hi, scalar2=0.5, op0=ALU.add, op1=ALU.mult),
             reads=[b_bis], writes=[b_bis])
        P.op(P.dve, lambda e: e.tensor_scalar(out=junk[:], in0=affS[:], scalar1=mid, scalar2=None, op0=ALU.is_ge,
                                              op1=ALU.add, accum_out=cn), reads=[b_bis, b_affS], writes=[b_bis, b_junk])
        bk, bbk = nbank()
        P.op(P.pe, lambda e: e.matmul(bk[:, 0:1], lhsT=BD[:], rhs=cn, start=True, stop=True),
             reads=[b_BD, b_bis], writes=[bbk])
        P.op(P.dve, lambda e: e.tensor_scalar(out=ge, in0=bk[:, 0:1], scalar1=float(CAP) - 0.5, scalar2=None,
                                              op0=ALU.is_ge), reads=[bbk, b_bis], writes=[b_bis])
        P.op(P.dve, lambda e: e.tensor_tensor(out=dd, in0=mid, in1=lo, op=ALU.subtract), reads=[b_bis], writes=[b_bis])
        P.op(P.dve, lambda e: e.scalar_tensor_tensor(out=lo, in0=dd, scalar=ge, in1=lo, op0=ALU.mult, op1=ALU.add),
             reads=[b_bis], writes=[b_bis])
        P.op(P.dve, lambda e: e.tensor_tensor(out=dd, in0=hi, in1=mid, op=ALU.subtract), reads=[b_bis], writes=[b_bis])
        P.op(P.dve, lambda e: e.scalar_tensor_tensor(out=hi, in0=dd, scalar=ge, in1=mid, op0=ALU.mult, op1=ALU.add),
             reads=[b_bis], writes=[b_bis])
    P.op(P.dve, lambda e: e.tensor_scalar(out=junk[:], in0=affS[:], scalar1=lo, scalar2=None, op0=ALU.is_ge),
         reads=[b_bis, b_affS], writes=[b_junk])
    P.op(P.dve, lambda e: e.tensor_tensor_scan(out=Cs[:], data0=onesS[:], data1=junk[:], initial=0.0,
                                               op0=ALU.mult, op1=ALU.add), reads=[b_ones, b_junk], writes=[b_Cs])
    bk, bbk = nbank()
    P.op(P.pe, lambda e: e.matmul(bk[:, 0:1], lhsT=LT[:], rhs=Cs[:, SG - 1:SG], start=True, stop=True),
         reads=[b_BD, b_Cs], writes=[bbk])
    P.op(P.dve, lambda e: e.tensor_copy(out=bis[:, 6:7], in_=bk[:, 0:1]), reads=[bbk, b_bis], writes=[b_bis])
    P.op(P.dve, lambda e: e.tensor_scalar(out=Cs[:], in0=Cs[:], scalar1=bis[:, 6:7], scalar2=None, op0=ALU.add),
         reads=[b_bis, b_Cs], writes=[b_Cs])
    b_Cd = B("Cd")
    P.dma(P.sp, lambda e: e.dma_start(out=C_d.rearrange("e (g n) -> (e g) n", g=G8), in_=Cs[:]), b_Cs,
          reads=[b_Cs], writes=[b_Cd])
    C3 = C_d.rearrange("e (t p) -> t e p", p=128)
    Ct = P.sb("Ct", [NT, 16, 129], F32); b_Ct = B("Ct")
    At = P.sb("At", [NT, 16, 128], F32); b_At = B("At")
    CTp = P.sb("CTp", [NT, 16, 1], F32); b_CTp = B("CTp")
    P.dma(P.sp, lambda e: e.dma_start(out=Ct[:, :, 0:128], in_=C3), b_Ct, reads=[b_Cd], writes=[b_Ct])
    P.op(P.pool, lambda e: e.iota(Ct[:, :, 128:129], pattern=[[0, 16]], base=0, channel_multiplier=128,
                                  allow_small_or_imprecise_dtypes=True), reads=[b_Ct], writes=[b_Ct])
    P.dma(P.sp, lambda e: e.dma_start(out=At[:], in_=affT_d.rearrange("e (t p) -> t e p", p=128)), b_At,
          reads=[B_affT], writes=[b_At])
    P.op(P.dve, lambda e: e.memset(CTp[:], 0.0), writes=[b_CTp])
    P.dma(P.sp, lambda e: e.dma_start(out=CTp[1:NT, :, :], in_=C3[0:NT - 1, :, 127:128], allow_slow_non_contiguous=True), b_CTp,
          reads=[b_Cd], writes=[b_CTp])
    iota_c = P.sb("iota_c", [NT, CAP], F32); cg = P.sb("cg", [128, NJ], F32); iota_p = P.sb("iota_p", [128, 128], F32)
    b_iota = B("iota")
    P.op(P.pool, lambda e: e.iota(iota_c[:], pattern=[[1, CAP]], base=0, channel_multiplier=0,
                                  allow_small_or_imprecise_dtypes=True), writes=[b_iota])
    P.op(P.pool, lambda e: e.iota(cg[:], pattern=[[128, NJ]], base=0, channel_multiplier=1,
                                  allow_small_or_imprecise_dtypes=True), writes=[b_iota])
    P.op(P.pool, lambda e: e.iota(iota_p[:], pattern=[[1, 128]], base=0, channel_multiplier=0,
                                  allow_small_or_imprecise_dtypes=True), writes=[b_iota])
    a_t = P.sb("a_t", [NT, CAP], F32); oh = P.sb("oh", [NT, CAP], F32); b_at = B("a_t"); b_oh = B("oh")
    jk = [P.sb(f"jk{i}", [128, 128], F32) for i in range(2)]; b_jk = [B(f"jk{i}") for i in range(2)]
    nloc = P.sb("nloc", [128, 2], F32); b_nloc = B("nloc")
    for ex in range(16):
        P.op(P.dve, lambda e: e.tensor_scalar(out=a_t[:], in0=iota_c[:], scalar1=CTp[:, ex, :], scalar2=None,
                                              op0=ALU.is_lt), reads=[b_iota, b_CTp], writes=[b_at])
        P.op(P.dve, lambda e: e.scalar_tensor_tensor(out=oh[:], in0=iota_c[:], scalar=Ct[:, ex, 127:128], in1=a_t[:],
                                                     op0=ALU.is_lt, op1=ALU.subtract),
             reads=[b_iota, b_Ct, b_at], writes=[b_oh])
        for j in range(NJ):
            bk, bbk = nbank()
            P.op(P.pe, lambda e: e.matmul(bk[:, 0:129], lhsT=oh[:, j * 128:(j + 1) * 128], rhs=Ct[:, ex, :],
                                          start=True, stop=True), reads=[b_oh, b_Ct], writes=[bbk])
            P.op(P.pe, lambda e: e.matmul(bk[:, 256:384], lhsT=oh[:, j * 128:(j + 1) * 128], rhs=At[:, ex, :],
                                          start=True, stop=True), reads=[b_oh, b_At], writes=[bbk])
            P.op(P.dve, lambda e: e.tensor_scalar(out=jk[0][:], in0=bk[:, 0:128], scalar1=cg[:, j:j + 1], scalar2=None,
                                                  op0=ALU.is_le, op1=ALU.add, accum_out=nloc[:, 0:1]),
                 reads=[bbk, b_iota], writes=[b_jk[0], b_nloc])
            P.op(P.dve, lambda e: e.tensor_tensor(out=idx_all[:, ex, j:j + 1], in0=bk[:, 128:129], in1=nloc[:, 0:1],
                                                  op=ALU.add), reads=[bbk, b_nloc], writes=[b_idx])
            P.op(P.dve, lambda e: e.scalar_tensor_tensor(out=jk[1][:], in0=iota_p[:], scalar=nloc[:, 0:1],
                                                         in1=bk[:, 256:384], op0=ALU.is_equal, op1=ALU.mult,
                                                         accum_out=gates[:, ex, j:j + 1]),
                 reads=[bbk, b_nloc, b_iota], writes=[b_jk[1], b_gates])
    pop()
    if stop_after == "B":
        return nc

    push()
    TH = max(1, CAP // 512); TN = CAP // TH
    XT = [P.sb(f"XT{i}", [128, 8, CAP], BF16) for i in range(2)]; b_XT = [B(f"XT{i}") for i in range(2)]
    actT = P.sb("actT", [128, 16, CAP], BF16); b_actT = B("actT")
    Xg = [P.sb(f"Xg{i}", [128, D], BF16) for i in range(3)]; b_Xg = [B(f"Xg{i}") for i in range(3)]
    sg = [P.sb(f"sg{i}", [128, 512], F32) for i in range(2)]; b_sg = [B(f"sg{i}") for i in range(2)]
    ysb = [P.sb(f"ysb{i}", [128, D], F32) for i in range(3)]; b_ysb = [B(f"ysb{i}") for i in range(3)]
    Wv = [B(f"wave{i}") for i in range(16)]
    B_scat = B("scat")
    cc = {"xg": 0, "sg": 0, "y": 0}

    def gather(ex):
        xt = XT[ex % 2]; bxt = b_XT[ex % 2]
        for j in range(NJ):
            sl = cc["xg"] % 3; cc["xg"] += 1
            P.dma(P.pool, lambda e: e.indirect_dma_start(out=Xg[sl][:, :], out_offset=None, in_=h16_d[:, :],
                                                         in_offset=bass.IndirectOffsetOnAxis(ap=idx_all[:, ex, j:j + 1], axis=0)),
                  b_Xg[sl], reads=[b_idx, B_h16], writes=[b_Xg[sl]])
            bk, bbk = nbank()
            bkb = bk[:].bitcast(BF16)
            for k in range(8):
                P.op(P.pe, lambda e: e.transpose(out=bkb[:, k * 128:(k + 1) * 128], in_=Xg[sl][:, k * 128:(k + 1) * 128],
                                                 identity=identb[:]), reads=[b_Xg[sl], b_id], writes=[bbk])
            P.op(P.act, lambda e: e.copy(out=xt[:, :, j * 128:(j + 1) * 128],
                                         in_=bkb[:, 0:1024].rearrange("p (k n) -> p k n", n=128)),
                 reads=[bbk], writes=[bxt])

    gather(0)
    for ex in range(16):
        xt = XT[ex % 2]; bxt = b_XT[ex % 2]
        for fb in range(4):
            n = 4 * ex + fb
            if n + 2 < 64:
                load_gu(n + 2)
            sl = n % 3
            for f4 in range(4):
                fc = 4 * fb + f4
                for th in range(TH):
                    bkG, bbG = nbank()
                    for k in range(8):
                        P.op(P.pe, lambda e: e.matmul(bkG[:, 0:TN], lhsT=Wg_r[sl][:, k, f4 * 128:(f4 + 1) * 128],
                                                      rhs=xt[:, k, th * TN:(th + 1) * TN], start=(k == 0), stop=(k == 7)),
                             reads=[b_Wg[sl], bxt], writes=[bbG])
                    bkU, bbU = nbank()
                    for k in range(8):
                        P.op(P.pe, lambda e: e.matmul(bkU[:, 0:TN], lhsT=Wu_r[sl][:, k, f4 * 128:(f4 + 1) * 128],
                                                      rhs=xt[:, k, th * TN:(th + 1) * TN], start=(k == 0), stop=(k == 7)),
                             reads=[b_Wu[sl], bxt], writes=[bbU])
                    si = cc["sg"] % 2; cc["sg"] += 1
                    P.op(P.act, lambda e: e.activation(out=sg[si][:, 0:TN], in_=bkG[:, 0:TN], func=AF.Silu),
                         reads=[bbG], writes=[b_sg[si]])
                    P.op(P.dve, lambda e: e.tensor_tensor(out=actT[:, fc, th * TN:(th + 1) * TN], in0=bkU[:, 0:TN],
                                                          in1=sg[si][:, 0:TN], op=ALU.mult),
                         reads=[bbU, b_sg[si]], writes=[b_actT])
        if ex + 1 < 16:
            gather(ex + 1)
        for j in range(NJ):
            yi = cc["y"] % 3; cc["y"] += 1
            for chh in range(2):
                bk, bbk = nbank()
                for fc in range(16):
                    P.op(P.pe, lambda e: e.matmul(bk[:, 0:512], lhsT=actT[:, fc, j * 128:(j + 1) * 128],
                                                  rhs=Wd_r[fc // 4][:, fc % 4, chh * 512:(chh + 1) * 512],
                                                  start=(fc == 0), stop=(fc == 15)),
                         reads=[b_actT, b_Wd[fc // 4]], writes=[bbk])
                P.op(P.act, lambda e: e.activation(out=ysb[yi][:, chh * 512:(chh + 1) * 512], in_=bk[:, 0:512],
                                                   func=AF.Identity, scale=gates[:, ex, j:j + 1]),
                     reads=[bbk, b_gates], writes=[b_ysb[yi]])
            prev = [Wv[ex - 1]] if ex > 0 else [B_acc]
            P.dma(P.pool, lambda e: e.indirect_dma_start(out=acc_d[:, :],
                                                         out_offset=bass.IndirectOffsetOnAxis(ap=idx_all[:, ex, j:j + 1], axis=0),
                                                         in_=ysb[yi][:, :], in_offset=None, compute_op=ALU.add),
                  b_ysb[yi], reads=[b_ysb[yi], b_idx] + prev, adds=[Wv[ex], B_scat])
        if ex + 1 < 16:
            load_d(ex + 1)
    pop()
    pop()
    if stop_after == "C":
        return nc

    push()
    ln2g = P.sb("ln2g", [128, D], F32); ln2b = P.sb("ln2b", [128, D], F32); b_ln2 = B("ln2")
    P.dma(P.sp, lambda e: e.dma_start(out=ln2g[:], in_=ln2g_d[:, :]), B("ln2g"), adds=[b_ln2])
    P.dma(P.sp, lambda e: e.dma_start(out=ln2b[:], in_=ln2b_d[:, :]), B("ln2b"), adds=[b_ln2])
    ND = 12
    at = [P.sb(f"at{i}", [128, D], F32) for i in range(ND)]; b_at2 = [B(f"at{i}") for i in range(ND)]
    ot = [P.sb(f"ot{i}", [128, D], F32) for i in range(ND)]; b_ot = [B(f"ot{i}") for i in range(ND)]
    def d_load(t):
        P.dma(P.sp, lambda e: e.dma_start(out=at[t % ND][:], in_=acc_d[t * 128:(t + 1) * 128, :]), b_at2[t % ND],
              reads=[B_acc, B_scat], writes=[b_at2[t % ND]])

    for t in range(min(ND - 1, NT)):
        d_load(t)
    def d_norm(t):
        layer_norm(at[t % ND], b_at2[t % ND], ot[t % ND], b_ot[t % ND], ln2g, ln2b, b_ln2, split=True)

    d_norm(0)
    for t in range(NT):
        o = ot[t % ND]; bo = b_ot[t % ND]
        if t + ND - 1 < NT:
            d_load(t + ND - 1)
        if t + 1 < NT:
            d_norm(t + 1)
        ln_affine(o, bo, ln2g, ln2b, b_ln2)
        P.dma(P.sp, lambda e: e.dma_start(out=out_d[t * 128:(t + 1) * 128, :], in_=o[:]), bo, reads=[bo])
    pop()
    return nc


def _perm():
    qa = np.arange(0, 512)
    qb = np.concatenate([np.concatenate([1536 + np.arange(64 * i, 64 * i + 64),
                                         1536 + np.arange(64 * (i + 4), 64 * (i + 4) + 64)]) for i in range(4)])
    ga = np.arange(2304, 3328); gb = np.arange(3328, 4352)
    ka = np.arange(512, 1024); kb = np.arange(2048, 2176)
    va = np.arange(1024, 1536); vb = np.arange(2176, 2304)
    return np.concatenate([qa, qb, ga, gb, ka, kb, va, vb])


def _na_bias(rpb, t, kts, rows):
    q = np.arange(128); r = 2 * t + q // 64; c = q % 64
    r0 = np.clip(r - 4, 0, rows - 8); cs = np.clip(c - 8, 0, 48)
    k = np.arange(128)
    out = np.full((128, 8, len(kts), 128), NEG, np.float32)
    for i, kt in enumerate(kts):
        rk = 2 * kt + k // 64; ck = k % 64
        valid = ((rk[:, None] >= r0[None, :]) & (rk[:, None] < r0[None, :] + 8)
                 & (ck[:, None] >= cs[None, :]) & (ck[:, None] < cs[None, :] + 16))
        dr = np.clip(rk[:, None] - r[None, :] + 7, 0, 14)
        dc = np.clip(ck[:, None] - c[None, :] + 15, 0, 30)
        vals = rpb[:, dr, dc]
        out[:, :, i, :] = np.where(valid[:, None, :], vals.transpose(1, 0, 2), np.float32(NEG))
    return np.ascontiguousarray(out[:, HORD]).reshape(128, 8 * len(kts) * 128)


def _kts(t, NT):
    if t < 2:
        return [0, 1, 2, 3]
    if t >= NT - 2:
        return [NT - 4, NT - 3, NT - 2, NT - 1]
    return [t - 2, t - 1, t, t + 1, t + 2]


def _wg_bias():
    k = np.arange(128)[:, None]; q = np.arange(128)[None, :]
    out = np.zeros((128, 3, 2, 4, 128), np.float32)
    for di, dt in enumerate((-1, 0, 1)):
        dist = np.abs(q - k - 128 * dt).astype(np.float32)
        for j in range(2):
            for g in range(4):
                slope = np.float32(2.0 ** (-(4 * j + g + 1)))
                out[:, di, j, g, :] = np.where(dist <= 128, -slope * dist, np.float32(NEG))
    return out.reshape(128, 24 * 128)


def _prep_shared(inp, S):
    NT = S // 128; rows = S // 64
    f = lambda a: np.ascontiguousarray(np.asarray(a), dtype=np.float32)
    perm = _perm()
    w_in = f(inp["w_in"])[0]; b_in = f(inp["b_in"])[0]
    b_p = b_in[perm]
    rpb = f(inp["rpb"])[0]
    rep = lambda v: np.ascontiguousarray(np.broadcast_to(v[None, :], (128, v.shape[0])))
    edge_tiles = [0, 1, NT - 2, NT - 1]
    sh = {
        "w_in_p": np.ascontiguousarray(w_in[:, perm]),
        "bfm": np.ascontiguousarray(b_p[:FMW].reshape(NQ, 128).T),
        "bv": rep(b_p[FMW:]),
        "nab_int": _na_bias(rpb, 2, _kts(2, NT), rows),
        "nab_edge": np.stack([_na_bias(rpb, t, _kts(t, NT), rows) for t in edge_tiles]),
        "wgb": _wg_bias(),
        "sinkb": rep(f(inp["sink"])[0]),
        "w_a": f(inp["w_branch_a"])[0], "w_b": f(inp["w_branch_b"])[0], "w_o": f(inp["w_out"])[0],
        "w_r": f(inp["w_router"])[0],
        "ln1g": rep(f(inp["ln1_g"])[0]), "ln1b": rep(f(inp["ln1_b"])[0]),
        "ln2g": rep(f(inp["ln2_g"])[0]), "ln2b": rep(f(inp["ln2_b"])[0]),
        "bd_c": (np.arange(128)[:, None] // 8 == np.arange(128)[None, :] // 8).astype(np.float32),
        "lt_c": ((np.arange(128)[:, None] // 8 == np.arange(128)[None, :] // 8)
                 & (np.arange(128)[:, None] < np.arange(128)[None, :])).astype(np.float32),
        "w_gate": f(inp["w_gate"])[0], "w_up": f(inp["w_up"])[0], "w_down": f(inp["w_down"])[0],
    }
    return sh


def _prep_core(xb):
    xb = np.ascontiguousarray(np.asarray(xb), dtype=np.float32)
    return {"xT": np.ascontiguousarray(xb.T), "x": xb}


def kernel(**inputs):
    x = np.asarray(inputs["x"])
    Bn, S, _ = x.shape
    nc = build(S)
    sh = _prep_shared(inputs, S)
    in_maps = []
    for b in range(Bn):
        m = dict(sh)
        m.update(_prep_core(x[b]))
        in_maps.append(m)
    res = run_bass_kernel_spmd(nc, in_maps, core_ids=list(range(Bn)))
    return np.stack([np.asarray(r["out"]) for r in res.results], axis=0).astype(np.float32)
```

```python
import numpy as np
import concourse.bass as bass
import concourse.mybir as mybir
from concourse.bass_utils import run_bass_kernel_spmd

F32 = mybir.dt.float32
BF16 = mybir.dt.bfloat16
I32 = mybir.dt.int32
AF = mybir.ActivationFunctionType
ALU = mybir.AluOpType
AX = mybir.AxisListType

EPOCH = 30000


class Buf:
    __slots__ = ("name", "w", "r", "dsem", "dcnt", "dcls")

    def __init__(self, name):
        self.name = name
        self.w = {}
        self.r = {}
        self.dsem = None
        self.dcnt = 0
        self.dcls = None


class Eng:
    def __init__(self, prog, name, eng):
        self.prog, self.name, self.eng = prog, name, eng
        self.n = 0
        self.sems = []
        self.seen = {}
        self.own = set()

    def next_event(self):
        ep, off = divmod(self.n, EPOCH)
        while len(self.sems) <= ep:
            s = self.prog.new_sem(f"{self.name}_e{len(self.sems)}")
            self.sems.append(s)
            self.own.add(id(s))
        self.n += 1
        return (self.sems[ep], off + 1)


class Prog:
    def __init__(self, nc):
        self.nc = nc
        self._keep = []
        self._sems = []
        self.free_sems = {}
        self.nsem = 0
        self.pe = Eng(self, "pe", nc.tensor)
        self.act = Eng(self, "act", nc.scalar)
        self.dve = Eng(self, "dve", nc.vector)
        self.pool = Eng(self, "pool", nc.gpsimd)
        self.sp = Eng(self, "sp", nc.sync)
        self.engs = [self.pe, self.act, self.dve, self.pool, self.sp]

    def new_sem(self, name):
        cm = self.nc.semaphore(name)
        s = cm.__enter__()
        self._sems.append(cm)
        self.nsem += 1
        return s

    def sb(self, name, shape, dt):
        cm = self.nc.sbuf_tensor("s_" + name, list(shape), dt)
        t = cm.__enter__()
        self._keep.append(cm)
        return t

    def ps(self, name, shape, dt):
        cm = self.nc.psum_tensor("p_" + name, list(shape), dt)
        t = cm.__enter__()
        self._keep.append(cm)
        return t

    def _deps(self, E, reads, writes):
        need = {}

        def add(ev, raw):
            sem, c = ev
            if id(sem) in E.own and not raw and E is self.pe:
                return
            k = id(sem)
            if k not in need or need[k][1] < c:
                need[k] = (sem, c)

        for b in reads:
            for ev in b.w.values():
                add(ev, True)
        for b in writes:
            for ev in b.w.values():
                add(ev, False)
            for ev in b.r.values():
                add(ev, False)
        for k, (sem, c) in need.items():
            if E.seen.get(k, 0) < c:
                E.eng.wait_ge(sem, c)
                E.seen[k] = c

    def _commit(self, ev, reads, writes):
        k = id(ev[0])
        for b in writes:
            b.w = {k: ev}
            b.r = {}
        for b in reads:
            b.r[k] = ev

    def op(self, E, fn, reads=(), writes=()):
        self._deps(E, reads, writes)
        ins = fn(E.eng)
        ev = E.next_event()
        ins.then_inc(ev[0], 1)
        self._commit(ev, reads, writes)
        return ins

    def dma(self, E, fn, semb, reads=(), writes=(), adds=()):
        self._deps(E, reads, writes)
        if semb.dsem is None:
            pool_ = self.free_sems.setdefault(E.name, [])
            if pool_:
                semb.dsem, semb.dcnt = pool_.pop()
            else:
                semb.dsem = self.new_sem("d_" + semb.name)
            semb.dcls = E.name
        assert semb.dcls == E.name, (semb.name, semb.dcls, E.name)
        ins = fn(E.eng)
        semb.dcnt += 16
        ev = (semb.dsem, semb.dcnt)
        ins.then_inc(ev[0], 16)
        self._commit(ev, reads, writes)
        for b in adds:
            b.w[id(ev[0])] = ev
        return ins

    def barrier_all(self, bufs):
        self._deps(self.sp, bufs, bufs)

    def full_barrier(self):
        evs = []
        for F in self.engs:
            if F.n > 0:
                ep, off = divmod(F.n - 1, EPOCH)
                evs.append((F.sems[ep], off + 1))
        for b in self.dbufs:
            if b.dsem is not None and b.dcnt > 0:
                evs.append((b.dsem, b.dcnt))
        for E in self.engs:
            for sem, c in evs:
                k = id(sem)
                if E.seen.get(k, 0) < c:
                    E.eng.wait_ge(sem, c)
                    E.seen[k] = c


HORD = [0, 2, 4, 6, 1, 3, 5, 7]
D = 1024
NQ = 29
FMW = NQ * 128
INW = 4352
ALPHA = 2.0 ** 0.25
NEG = -30000.0


def build(S, debug=False, stop_after=None):
    NT = S // 128
    NS = S // 512
    CAP = S // 8
    NJ = CAP // 128
    nc = bass.Bass("TRN2", target_bir_lowering=False)
    P = Prog(nc)
    P.dbufs = []

    def B(name):
        b = Buf(name)
        P.dbufs.append(b)
        return b

    def din(name, shape, dt=F32):
        return nc.dram_tensor(name, list(shape), dt, kind="ExternalInput").ap()

    def dscr(name, shape, dt):
        return nc.dram_tensor(name, list(shape), dt, kind="ExternalOutput" if debug else "Internal").ap()

    xT_d = din("xT", [D, S])
    x_d = din("x", [S, D])
    win_d = din("w_in_p", [D, INW])
    bfm_d = din("bfm", [128, NQ])
    bv_d = din("bv", [128, 640])
    nabi_d = din("nab_int", [128, 40 * 128])
    nabe_d = din("nab_edge", [4, 128, 32 * 128])
    wgb_d = din("wgb", [128, 24 * 128])
    sink_d = din("sinkb", [128, 8])
    wa_d = din("w_a", [512, D])
    wb_d = din("w_b", [512, D])
    wo_d = din("w_o", [D, D])
    wr_d = din("w_r", [D, 16])
    ln1g_d = din("ln1g", [128, D]); ln1b_d = din("ln1b", [128, D])
    ln2g_d = din("ln2g", [128, D]); ln2b_d = din("ln2b", [128, D])
    wg_d = din("w_gate", [16, D, 2048])
    wu_d = din("w_up", [16, D, 2048])
    wd_d = din("w_down", [16, 2048, D])
    bd_d = din("bd_c", [128, 128]); lt_d = din("lt_c", [128, 128])
    out_d = nc.dram_tensor("out", [S, D], F32, kind="ExternalOutput").ap()

    pfm_d = dscr("pfm", [NT, 128, FMW], BF16)
    vtm_d = dscr("vtm", [S, 640], BF16)
    h16_d = dscr("h16", [S, D], BF16)
    acc_d = dscr("acc", [S, D], F32)
    affT_d = dscr("affT", [16, S], F32)
    C_d = dscr("Ccum", [16, S], F32)

    banks = [P.ps(f"bank{i}", [128, 512], F32) for i in range(8)]
    bbank = [B(f"bank{i}") for i in range(8)]
    rot = {"i": 0, "n": 8}

    busy = [False] * 8
    rot["strict"] = False

    def nbank():
        for _ in range(rot["n"]):
            i = rot["i"] % rot["n"]
            rot["i"] += 1
            if not busy[i]:
                break
        else:
            raise RuntimeError("no free PSUM bank")
        if rot["strict"]:
            busy[i] = True
        return banks[i], bbank[i]

    def rel(bb):
        busy[bbank.index(bb)] = False

    identb = P.sb("identb", [128, 128], BF16)
    identf = P.sb("identf", [128, 128], F32)
    b_id = B("ident")
    P.op(P.pool, lambda e: e.memset(identf[:], 0.0), writes=[b_id])
    P.op(P.pool, lambda e: e.affine_select(out=identf[:], in_=identf[:], pattern=[[-1, 128]],
                                           compare_op=ALU.not_equal, fill=1.0, base=0,
                                           channel_multiplier=1), reads=[b_id], writes=[b_id])
    P.op(P.dve, lambda e: e.tensor_copy(out=identb[:], in_=identf[:]), reads=[b_id], writes=[b_id])

    LNS = 12
    stats_l = [P.sb(f"stats{i}", [128, 2, 6], F32) for i in range(LNS)]; b_stats_l = [B(f"stats{i}") for i in range(LNS)]
    mv_l = [P.sb(f"mv{i}", [128, 2], F32) for i in range(LNS)]; b_mv_l = [B(f"mv{i}") for i in range(LNS)]
    lnv_l = [P.sb(f"lnv{i}", [128, 4], F32) for i in range(LNS)]; b_lnv_l = [B(f"lnv{i}") for i in range(LNS)]
    lnc = {"i": 0}
    mhalf = P.sb("mhalf", [128, 1], F32); b_mhalf = B("mhalf")
    P.op(P.pool, lambda e: e.memset(mhalf[:], -0.5), writes=[b_mhalf])
    mone = P.sb("mone", [128, 1], F32)
    P.op(P.pool, lambda e: e.memset(mone[:], -1.0), writes=[b_mhalf])
    idx_all = P.sb("idx_all", [128, 16, NJ], I32); gates = P.sb("gates", [128, 16, NJ], F32)
    b_idx = B("idx_all"); b_gates = B("gates")
    B_h16 = B("h16d"); B_acc = B("accd"); B_affT = B("affTd"); B_pfm = B("pfm"); B_vtm = B("vtm")

    scopes = []

    def push():
        scopes.append((len(P._keep), len(P.dbufs)))

    def pop():
        P.full_barrier()
        n, nb = scopes.pop()
        while len(P._keep) > n:
            P._keep.pop().__exit__(None, None, None)
        for b in P.dbufs[nb:]:
            if b.dsem is not None:
                P.free_sems.setdefault(b.dcls, []).append((b.dsem, b.dcnt))
                b.dsem = None
        del P.dbufs[nb:]

    push()
    W = P.sb("W_in", [128, 8, INW], BF16); b_W = B("W_in")
    win_v = win_d.rearrange("(k p) n -> p k n", p=128)
    xs = [P.sb(f"xs{i}", [128, 8, 512], BF16) for i in range(2)]
    b_xs = [B(f"xs{i}") for i in range(2)]
    xT_v = xT_d.rearrange("(k p) n -> p k n", p=128)

    def a1_load(s):
        P.dma(P.pool, lambda e: e.dma_start(out=xs[s % 2][:], in_=xT_v[:, :, s * 512:(s + 1) * 512]),
              b_xs[s % 2], writes=[b_xs[s % 2]])

    b_Wb_ = []
    for bi in range((INW + 511) // 512):
        c0, c1 = bi * 512, min(INW, bi * 512 + 512)
        bb = B(f"Wld{bi}")
        P.dma(P.pool, lambda e: e.dma_start(out=W[:, :, c0:c1], in_=win_v[:, :, c0:c1]), bb, writes=[bb])
        b_Wb_.append(bb)
        if bi == 0:
            a1_load(0)

    def wdeps(c0, c1):
        return [b_Wb_[i] for i in range(c0 // 512, (c1 - 1) // 512 + 1)]
    bfm = P.sb("bfm", [128, NQ], F32); bq8 = P.sb("bq8", [128, 8], F32); b_bfm = B("bfm")
    bv = P.sb("bv", [128, 640], F32); b_bv = B("bv")
    P.dma(P.sp, lambda e: e.dma_start(out=bfm[:], in_=bfm_d[:, :]), b_bfm, writes=[b_bfm])
    P.dma(P.sp, lambda e: e.dma_start(out=bv[:], in_=bv_d[:, :]), b_bv, writes=[b_bv])
    b_bq8 = B("bq8")
    P.op(P.dve, lambda e: e.tensor_scalar(out=bq8[:], in0=bfm[:, 0:8], scalar1=0.125, scalar2=None,
                                          op0=ALU.mult), reads=[b_bfm], writes=[b_bq8])
    stage = [P.sb(f"stage{i}", [128, 4, NQ, 128], BF16) for i in range(2)]
    b_stage = [B(f"stage{i}") for i in range(2)]
    vst = [P.sb(f"vst{i}", [128, 4, 640], BF16) for i in range(2)]
    b_vst = [B(f"vst{i}") for i in range(2)]
    for s in range(NS):
        if s + 1 < NS:
            a1_load(s + 1)
        xb, bx = xs[s % 2], b_xs[s % 2]
        st, bst = stage[s % 2], b_stage[s % 2]
        vs, bvs = vst[s % 2], b_vst[s % 2]
        for c in range(NQ):
            bk, bbk = nbank()
            for k in range(8):
                P.op(P.pe, lambda e: e.matmul(bk[:, 0:512], lhsT=W[:, k, c * 128:(c + 1) * 128],
                                              rhs=xb[:, k, :], start=(k == 0), stop=(k == 7)),
                     reads=wdeps(c * 128, c * 128 + 128) + [bx], writes=[bbk])
            src = bk[:, 0:512].rearrange("p (t n) -> p t n", n=128)
            if c < 8:
                P.op(P.act, lambda e: e.activation(out=st[:, :, c, :], in_=src, func=AF.Identity,
                                                   bias=bq8[:, c:c + 1], scale=0.125),
                     reads=[bbk, b_bq8], writes=[bst])
            elif c < 24:
                P.op(P.act, lambda e: e.activation(out=st[:, :, c, :], in_=src, func=AF.Sigmoid,
                                                   bias=bfm[:, c:c + 1], scale=1.0),
                     reads=[bbk, b_bfm], writes=[bst])
            else:
                P.op(P.act, lambda e: e.activation(out=st[:, :, c, :], in_=src, func=AF.Identity,
                                                   bias=bfm[:, c:c + 1], scale=1.0),
                     reads=[bbk, b_bfm], writes=[bst])
        for i in range(4):
            bk, bbk = nbank()
            for k in range(8):
                P.op(P.pe, lambda e: e.matmul(bk[:, 0:512], lhsT=xb[:, k, i * 128:(i + 1) * 128],
                                              rhs=W[:, k, FMW:FMW + 512], start=(k == 0), stop=(k == 7)),
                     reads=wdeps(FMW, FMW + 512) + [bx], writes=[bbk])
            P.op(P.dve, lambda e: e.tensor_tensor(out=vs[:, i, 0:512], in0=bk[:, 0:512], in1=bv[:, 0:512],
                                                  op=ALU.add), reads=[bbk, b_bv], writes=[bvs])
            bk, bbk = nbank()
            for k in range(8):
                P.op(P.pe, lambda e: e.matmul(bk[:, 0:128], lhsT=xb[:, k, i * 128:(i + 1) * 128],
                                              rhs=W[:, k, FMW + 512:INW], start=(k == 0), stop=(k == 7)),
                     reads=wdeps(FMW + 512, INW) + [bx], writes=[bbk])
            P.op(P.dve, lambda e: e.tensor_tensor(out=vs[:, i, 512:640], in0=bk[:, 0:128], in1=bv[:, 512:640],
                                                  op=ALU.add), reads=[bbk, b_bv], writes=[bvs])
        P.dma(P.sp, lambda e: e.dma_start(
            out=pfm_d[4 * s:4 * s + 4].rearrange("t p f -> p t f"),
            in_=st[:].rearrange("p t c n -> p t (c n)")), bst, reads=[bst], adds=[B_pfm])
        P.dma(P.sp, lambda e: e.dma_start(
            out=vtm_d.rearrange("(t p) f -> p t f", p=128)[:, 4 * s:4 * s + 4, :],
            in_=vs[:]), bvs, reads=[bvs], adds=[B_vtm])
    pop()
    if stop_after == "A1":
        return nc

    push()
    rot["n"] = 8; rot["i"] = 0; rot["strict"] = True
    Wa = P.sb("Wa", [128, 4, D], BF16); Wb = P.sb("Wb", [128, 4, D], BF16); Wo = P.sb("Wo", [128, 8, D], BF16)
    Wr = P.sb("Wr", [128, 8, 16], F32)
    b_Wa = B("Wa"); b_Wb = B("Wb"); b_Wo = B("Wo"); b_Wr = B("Wr")
    def load_merge_w():
        P.dma(P.pool, lambda e: e.dma_start(out=Wa[:], in_=wa_d.rearrange("(k p) n -> p k n", p=128)), b_Wa, writes=[b_Wa])
        P.dma(P.pool, lambda e: e.dma_start(out=Wb[:], in_=wb_d.rearrange("(k p) n -> p k n", p=128)), b_Wb, writes=[b_Wb])
        P.dma(P.pool, lambda e: e.dma_start(out=Wo[:], in_=wo_d.rearrange("(k p) n -> p k n", p=128)), b_Wo, writes=[b_Wo])
    P.dma(P.sp, lambda e: e.dma_start(out=Wr[:], in_=wr_d.rearrange("(k p) n -> p k n", p=128)), b_Wr, writes=[b_Wr])
    nabi = P.sb("nabi", [128, 40 * 128], BF16); b_nabi = B("nabi")
    nabe = P.sb("nabe", [128, 32 * 128], BF16); b_nabe = B("nabe")
    wgb = P.sb("wgb", [128, 24 * 128], BF16); b_wgb = B("wgb")
    for c0 in range(0, 40 * 128, 2048):
        c1 = min(c0 + 2048, 40 * 128)
        P.dma(P.pool, lambda e: e.dma_start(out=nabi[:, c0:c1], in_=nabi_d[:, c0:c1]), B(f"nabi{c0}"), adds=[b_nabi])
    for c0 in range(0, 24 * 128, 1536):
        P.dma(P.pool, lambda e: e.dma_start(out=wgb[:, c0:c0 + 1536], in_=wgb_d[:, c0:c0 + 1536]), B(f"wgb{c0}"), adds=[b_wgb])
    ln1g = P.sb("ln1g", [128, D], F32); ln1b = P.sb("ln1b", [128, D], F32); b_ln1 = B("ln1")
    P.dma(P.sp, lambda e: e.dma_start(out=ln1g[:], in_=ln1g_d[:, :]), B("ln1g"), adds=[b_ln1])
    P.dma(P.sp, lambda e: e.dma_start(out=ln1b[:], in_=ln1b_d[:, :]), B("ln1b"), adds=[b_ln1])
    esink = P.sb("esink", [128, 8, 1], F32); b_esink = B("esink")
    P.dma(P.sp, lambda e: e.dma_start(out=esink[:].rearrange("p h o -> p (h o)"), in_=sink_d[:, :]), b_esink, writes=[b_esink])
    P.op(P.act, lambda e: e.activation(out=esink[:], in_=esink[:], func=AF.Exp), reads=[b_esink], writes=[b_esink])

    Q2 = [P.sb(f"Q2_{i}", [128, 8, 128], BF16) for i in range(2)]; b_Q2 = [B(f"Q2_{i}") for i in range(2)]
    G4 = [P.sb(f"G4_{i}", [128, 16, 128], BF16) for i in range(4)]; b_G4 = [B(f"G4_{i}") for i in range(4)]
    Kr = [P.sb(f"Kr{i}", [128, 5, 128], BF16) for i in range(8)]; b_K = [B(f"Kr{i}") for i in range(8)]
    Vr = [P.sb(f"Vr{i}", [128, 10, 65], BF16) for i in range(8)]; b_V = [B(f"Vr{i}") for i in range(8)]
    for i in range(8):
        P.op(P.pool, lambda e: e.memset(Vr[i][:, :, 64:65], 1.0), writes=[b_V[i]])
    xtok = [P.sb(f"xtok{i}", [128, D], F32) for i in range(2)]; b_xtok = [B(f"xtok{i}") for i in range(2)]
    sc = [P.sb(f"sc{i}", [128, 512], F32) for i in range(3)]; b_sc = [B(f"sc{i}") for i in range(3)]
    PTa2 = [P.sb(f"PTa{i}", [128, 40, 128], BF16) for i in range(2)]; b_PTa2 = [B(f"PTa{i}") for i in range(2)]
    PTb2 = [P.sb(f"PTb{i}", [128, 3, 2, 4, 128], BF16) for i in range(2)]; b_PTb2 = [B(f"PTb{i}") for i in range(2)]
    rden = P.sb("rden", [128, 16, 1], F32); b_rden = B("rden")
    ya = P.sb("ya", [128, 8, 64], BF16); yb = P.sb("yb", [128, 8, 64], BF16); b_ya = B("ya"); b_yb = B("yb")
    yT2 = [P.sb(f"yT{i}", [128, 8, 128], BF16) for i in range(2)]; b_yT2 = [B(f"yT{i}") for i in range(2)]
    t1 = P.sb("t1", [128, 512], F32); t2 = P.sb("t2", [128, 512], F32); b_t1 = B("t1"); b_t2 = B("t2")
    mixT2 = [P.sb(f"mixT{i}", [128, 8, 128], BF16) for i in range(2)]; b_mixT2 = [B(f"mixT{i}") for i in range(2)]
    rr = [P.sb(f"rr{i}", [128, D], F32) for i in range(2)]; b_rr = [B(f"rr{i}") for i in range(2)]
    hh = [P.sb(f"hh{i}", [128, D], F32) for i in range(2)]; b_hh = [B(f"hh{i}") for i in range(2)]
    h16 = [P.sb(f"h16_{i}", [128, D], BF16) for i in range(2)]; b_h16s = [B(f"h16_{i}") for i in range(2)]
    hT = P.sb("hT", [128, 8, 128], F32); b_hT = B("hT")
    sm = P.sb("sm", [128, 4], F32); b_sm = B("sm")
    e16 = P.sb("e16", [128, 16], F32); aff = P.sb("aff", [128, 16], F32); b_e16 = B("e16"); b_aff = B("aff")
    affTs = [P.sb(f"affTs{i}", [16, 128], F32) for i in range(2)]; b_affTs = [B(f"affTs{i}") for i in range(2)]
    cnt = {"sc": 0}
    edge_tiles = [0, 1, NT - 2, NT - 1]

    def load_kv(kt):
        sl = kt % 8
        P.dma(P.sp, lambda e: e.dma_start(out=Kr[sl][:].rearrange("p c n -> p (c n)"), in_=pfm_d[kt, :, 3072:FMW]),
              b_K[sl], reads=[B_pfm], writes=[b_K[sl]])
        P.dma(P.sp, lambda e: e.dma_start(out=Vr[sl][:, :, 0:64],
                                          in_=vtm_d[kt * 128:(kt + 1) * 128, :].rearrange("p (h d) -> p h d", d=64)),
              b_V[sl], reads=[B_vtm], writes=[b_V[sl]])

    def load_q(t):
        sl = t % 2
        P.dma(P.sp, lambda e: e.dma_start(out=Q2[sl][:].rearrange("p c n -> p (c n)"), in_=pfm_d[t, :, 0:1024]),
              b_Q2[sl], reads=[B_pfm], writes=[b_Q2[sl]])
        g = t % 4
        P.dma(P.sp, lambda e: e.dma_start(out=G4[g][:].rearrange("p c n -> p (c n)"), in_=pfm_d[t, :, 1024:3072]),
              b_G4[g], reads=[B_pfm], writes=[b_G4[g]])

    def load_x(t):
        sl = t % 2
        P.dma(P.sp, lambda e: e.dma_start(out=xtok[sl][:], in_=x_d[t * 128:(t + 1) * 128, :]),
              b_xtok[sl], writes=[b_xtok[sl]])

    def stage1(t):
        kts = _kts(t, NT); nk = len(kts)
        q = Q2[t % 2]; bq = b_Q2[t % 2]
        PTa = PTa2[t % 2]; b_PTa = b_PTa2[t % 2]; PTb = PTb2[t % 2]; b_PTb = b_PTb2[t % 2]
        if nk == 5:
            nab, bnab = nabi, b_nabi
        else:
            ei = edge_tiles.index(t)
            for c0 in range(0, 32 * 128, 2048):
                P.dma(P.pool, lambda e: e.dma_start(out=nabe[:, c0:c0 + 2048], in_=nabe_d[ei, :, c0:c0 + 2048]),
                      B(f"nabe{t}_{c0}"), writes=[b_nabe] if c0 == 0 else [], adds=[b_nabe] if c0 else [])
            nab, bnab = nabe, b_nabe
        for uu in range(2 * nk):
            u = (uu // 2) + nk * (uu % 2)
            bk, bbk = nbank()
            for jj in range(4):
                hp, i = divmod(4 * u + jj, nk); h = HORD[hp]; kt = kts[i]; ch, half = divmod(h, 2)
                sl = slice(64 * half, 64 * half + 64)
                P.op(P.pe, lambda e: e.matmul(bk[:, jj * 128:(jj + 1) * 128], lhsT=Kr[kt % 8][sl, ch, :],
                                              rhs=q[sl, ch, :], start=True, stop=True),
                     reads=[b_K[kt % 8], bq], writes=[bbk])
            si = cnt["sc"] % 3; cnt["sc"] += 1
            P.op(P.dve, lambda e: e.tensor_tensor(out=sc[si][:], in0=bk[:, 0:512], in1=nab[:, u * 512:(u + 1) * 512],
                                                  op=ALU.add), reads=[bbk, bnab], writes=[b_sc[si]])
            rel(bbk)
            P.op(P.act, lambda e: e.activation(out=PTa[:, 4 * u:4 * u + 4, :].rearrange("p b n -> p (b n)"),
                                               in_=sc[si][:], func=AF.Exp), reads=[b_sc[si]], writes=[b_PTa])
            yield
        dts = [dt for dt in (-1, 0, 1) if 0 <= t + dt < NT]
        for j in range(2):
            sl = slice(64 * j, 64 * j + 64)
            for dt in dts:
                di = dt + 1; kt = t + dt
                bk, bbk = nbank()
                P.op(P.pe, lambda e: e.matmul(bk[:, 0:512], lhsT=Kr[kt % 8][sl, 4, :],
                                              rhs=q[sl, 4:8, :].rearrange("p c n -> p (c n)"), start=True, stop=True),
                     reads=[b_K[kt % 8], bq], writes=[bbk])
                si = cnt["sc"] % 3; cnt["sc"] += 1
                o = (di * 2 + j) * 512
                P.op(P.dve, lambda e: e.tensor_tensor(out=sc[si][:], in0=bk[:, 0:512], in1=wgb[:, o:o + 512],
                                                      op=ALU.add), reads=[bbk, b_wgb], writes=[b_sc[si]])
                rel(bbk)
                P.op(P.act, lambda e: e.activation(out=PTb[:, di, j].rearrange("p g n -> p (g n)"),
                                                   in_=sc[si][:], func=AF.Exp), reads=[b_sc[si]], writes=[b_PTb])
                yield

    def s2_pv(t):
        kts = _kts(t, NT); nk = len(kts)
        PTa = PTa2[t % 2]; b_PTa = b_PTa2[t % 2]; PTb = PTb2[t % 2]; b_PTb = b_PTb2[t % 2]
        dts = [dt for dt in (-1, 0, 1) if 0 <= t + dt < NT]
        for hb in range(2):
            pv, bpv = nbank()
            for h in range(4 * hb, 4 * hb + 4):
                col = (h % 4) * 65
                for i, kt in enumerate(kts):
                    P.op(P.pe, lambda e: e.matmul(pv[:, col:col + 65], lhsT=PTa[:, HORD.index(h) * nk + i, :],
                                                  rhs=Vr[kt % 8][:, h, :], start=(i == 0), stop=(i == nk - 1)),
                         reads=[b_PTa, b_V[kt % 8]], writes=[bpv])
                yield
            pvv = pv[:, 0:260].rearrange("p (h d) -> p h d", d=65)
            P.op(P.dve, lambda e: e.reciprocal(out=rden[:, 4 * hb:4 * hb + 4, :], in_=pvv[:, :, 64:65]),
                 reads=[bpv], writes=[b_rden])
            P.op(P.dve, lambda e: e.tensor_tensor(out=ya[:, 4 * hb:4 * hb + 4, :], in0=pvv[:, :, 0:64],
                                                  in1=rden[:, 4 * hb:4 * hb + 4, :].to_broadcast([128, 4, 64]),
                                                  op=ALU.mult), reads=[bpv, b_rden], writes=[b_ya])
            rel(bpv)
        for hb in range(2):
            pv, bpv = nbank()
            for hd in range(4 * hb, 4 * hb + 4):
                j, g = divmod(hd, 4); col = (hd % 4) * 65
                for ii, dt in enumerate(dts):
                    kt = t + dt
                    P.op(P.pe, lambda e: e.matmul(pv[:, col:col + 65], lhsT=PTb[:, dt + 1, j, g, :],
                                                  rhs=Vr[kt % 8][:, 8 + j, :], start=(ii == 0), stop=(ii == len(dts) - 1)),
                         reads=[b_PTb, b_V[kt % 8]], writes=[bpv])
                yield
            pvv = pv[:, 0:260].rearrange("p (h d) -> p h d", d=65)
            rs = rden[:, 8 + 4 * hb:12 + 4 * hb, :]
            P.op(P.dve, lambda e: e.tensor_tensor(out=rs, in0=pvv[:, :, 64:65], in1=esink[:, 4 * hb:4 * hb + 4, :],
                                                  op=ALU.add), reads=[bpv, b_esink], writes=[b_rden])
            P.op(P.dve, lambda e: e.reciprocal(out=rs, in_=rs), reads=[b_rden], writes=[b_rden])
            P.op(P.dve, lambda e: e.tensor_tensor(out=yb[:, 4 * hb:4 * hb + 4, :], in0=pvv[:, :, 0:64],
                                                  in1=rs.to_broadcast([128, 4, 64]), op=ALU.mult),
                 reads=[bpv, b_rden], writes=[b_yb])
            rel(bpv)

    def s2_tr(t):
        yT = yT2[t % 2]; b_yT = b_yT2[t % 2]
        bk, bbk = nbank()
        bkb = bk[:].bitcast(BF16)
        yaf = ya[:].rearrange("p h d -> p (h d)"); ybf = yb[:].rearrange("p h d -> p (h d)")
        for c in range(4):
            P.op(P.pe, lambda e: e.transpose(out=bkb[:, c * 128:(c + 1) * 128], in_=yaf[:, c * 128:(c + 1) * 128],
                                             identity=identb[:]), reads=[b_ya, b_id], writes=[bbk])
        for c in range(4):
            P.op(P.pe, lambda e: e.transpose(out=bkb[:, 512 + c * 128:512 + (c + 1) * 128],
                                             in_=ybf[:, c * 128:(c + 1) * 128], identity=identb[:]),
                 reads=[b_yb, b_id], writes=[bbk])
        P.op(P.act, lambda e: e.copy(out=yT[:].rearrange("p c n -> p (c n)"), in_=bkb[:, 0:1024]),
             reads=[bbk], writes=[b_yT])
        rel(bbk)
        yield

    def s2_br(t):
        yT = yT2[t % 2]; b_yT = b_yT2[t % 2]
        mixT = mixT2[t % 2]; b_mixT = b_mixT2[t % 2]
        g4 = G4[t % 4]; bq = b_G4[t % 4]
        for half in range(2):
            bkX, bbX = nbank()
            for c in range(4):
                for k in range(4):
                    P.op(P.pe, lambda e: e.matmul(bkX[:, c * 128:(c + 1) * 128],
                                                  lhsT=Wa[:, k, (4 * half + c) * 128:(4 * half + c + 1) * 128],
                                                  rhs=yT[:, k, :], start=(k == 0), stop=(k == 3)),
                         reads=[b_Wa, b_yT], writes=[bbX])
            yield
            bkY, bbY = nbank()
            for c in range(4):
                for k in range(4):
                    P.op(P.pe, lambda e: e.matmul(bkY[:, c * 128:(c + 1) * 128],
                                                  lhsT=Wb[:, k, (4 * half + c) * 128:(4 * half + c + 1) * 128],
                                                  rhs=yT[:, 4 + k, :], start=(k == 0), stop=(k == 3)),
                         reads=[b_Wb, b_yT], writes=[bbY])
            yield
            P.op(P.dve, lambda e: e.tensor_tensor(out=t1[:], in0=bkX[:, 0:512],
                                                  in1=g4[:, 4 * half:4 * half + 4, :].rearrange("p c n -> p (c n)"),
                                                  op=ALU.mult), reads=[bbX, bq], writes=[b_t1])
            rel(bbX)
            P.op(P.dve, lambda e: e.tensor_tensor(out=t2[:], in0=bkY[:, 0:512],
                                                  in1=g4[:, 8 + 4 * half:12 + 4 * half, :].rearrange("p c n -> p (c n)"),
                                                  op=ALU.mult), reads=[bbY, bq], writes=[b_t2])
            rel(bbY)
            P.op(P.pool, lambda e: e.tensor_tensor(out=mixT[:, 4 * half:4 * half + 4, :].rearrange("p c n -> p (c n)"),
                                                   in0=t1[:], in1=t2[:], op=ALU.add),
                 reads=[b_t1, b_t2], writes=[b_mixT])

    def s2_mix_b(t):
        mixT = mixT2[t % 2]; b_mixT = b_mixT2[t % 2]
        r = rr[t % 2]; br = b_rr[t % 2]
        xt = xtok[t % 2]; bxt = b_xtok[t % 2]
        for chh in range(2):
            bk, bbk = nbank()
            for k in range(8):
                P.op(P.pe, lambda e: e.matmul(bk[:, 0:512], lhsT=mixT[:, k, :], rhs=Wo[:, k, chh * 512:(chh + 1) * 512],
                                              start=(k == 0), stop=(k == 7)), reads=[b_mixT, b_Wo], writes=[bbk])
            P.op(P.dve, lambda e: e.scalar_tensor_tensor(out=r[:, chh * 512:(chh + 1) * 512],
                                                         in0=xt[:, chh * 512:(chh + 1) * 512], scalar=ALPHA,
                                                         in1=bk[:, 0:512], op0=ALU.mult, op1=ALU.add),
                 reads=[bbk, bxt], writes=[br])
            rel(bbk)
            yield

    def s2_ln(t):
        r = rr[t % 2]; br = b_rr[t % 2]; h = hh[t % 2]; bh = b_hh[t % 2]
        layer_norm(r, br, h, bh, ln1g, ln1b, b_ln1, g_on_pool=True)
        h6 = h16[t % 2]; bh6 = b_h16s[t % 2]
        P.op(P.act, lambda e: e.copy(out=h6[:], in_=h[:]), reads=[bh], writes=[bh6])
        P.op(P.act, lambda e: e.activation(out=r[:], in_=h[:], func=AF.Identity, scale=ALPHA), reads=[bh], writes=[br])
        P.dma(P.sp, lambda e: e.dma_start(out=h16_d[t * 128:(t + 1) * 128, :], in_=h6[:]), bh6, reads=[bh6], adds=[B_h16])
        P.dma(P.sp, lambda e: e.dma_start(out=acc_d[t * 128:(t + 1) * 128, :], in_=r[:]), br, reads=[br], adds=[B_acc])

    def s2_ln_b(t):
        h = hh[t % 2]; bh = b_hh[t % 2]
        for kb in range(2):
            bk, bbk = nbank()
            for k4 in range(4):
                k = 4 * kb + k4
                P.op(P.pe, lambda e: e.transpose(out=bk[:, k4 * 128:(k4 + 1) * 128], in_=h[:, k * 128:(k + 1) * 128],
                                                 identity=identf[:]), reads=[bh, b_id], writes=[bbk])
            P.op(P.act, lambda e: e.copy(out=hT[:, 4 * kb:4 * kb + 4, :].rearrange("p c n -> p (c n)"), in_=bk[:, 0:512]),
                 reads=[bbk], writes=[b_hT])
            rel(bbk)
            yield
        bk, bbk = nbank()
        for k in range(8):
            P.op(P.pe, lambda e: e.matmul(bk[:, 0:16], lhsT=hT[:, k, :], rhs=Wr[:, k, :], start=(k == 0), stop=(k == 7)),
                 reads=[b_hT, b_Wr], writes=[bbk])
        yield
        P.op(P.act, lambda e: e.activation(out=e16[:], in_=bk[:, 0:16], func=AF.Exp, accum_out=sm[:, 2:3]),
             reads=[bbk], writes=[b_e16, b_sm])
        rel(bbk)
        P.op(P.pool, lambda e: e.tensor_tensor(out=sm[:, 3:4], in0=sm[:, 2:3], in1=mone[:], op=ALU.pow),
             reads=[b_sm, b_mhalf], writes=[b_sm])
        P.op(P.pool, lambda e: e.tensor_scalar(out=aff[:], in0=e16[:], scalar1=sm[:, 3:4], scalar2=1.0,
                                               op0=ALU.mult, op1=ALU.mult), reads=[b_e16, b_sm], writes=[b_aff])
        bk, bbk = nbank()
        P.op(P.pe, lambda e: e.transpose(out=bk[0:16, 0:128], in_=aff[:, 0:16], identity=identf[:]),
             reads=[b_aff, b_id], writes=[bbk])
        ats = affTs[t % 2]; bats = b_affTs[t % 2]
        P.op(P.act, lambda e: e.copy(out=ats[:], in_=bk[0:16, 0:128]), reads=[bbk], writes=[bats])
        rel(bbk)
        P.dma(P.sp, lambda e: e.dma_start(out=affT_d[:, t * 128:(t + 1) * 128], in_=ats[:]), bats, reads=[bats], adds=[B_affT])

    def layer_norm(r, br, h, bh, g, b, bgb, g_on_pool=False, split=False):
        li = lnc["i"] % LNS; lnc["i"] += 1
        stats, mv, lnv = stats_l[li], mv_l[li], lnv_l[li]
        b_stats, b_mv, b_lnv = b_stats_l[li], b_mv_l[li], b_lnv_l[li]
        for c in range(2):
            P.op(P.dve, lambda e: e.bn_stats(out=stats[:, c, :], in_=r[:, c * 512:(c + 1) * 512]), reads=[br], writes=[b_stats])
        P.op(P.dve, lambda e: e.bn_aggr(out=mv[:], in_=stats[:].rearrange("p a b -> p (a b)")), reads=[b_stats], writes=[b_mv])
        P.op(P.dve, lambda e: e.tensor_scalar(out=lnv[:, 0:1], in0=mv[:, 1:2], scalar1=1e-5, scalar2=None, op0=ALU.add),
             reads=[b_mv], writes=[b_lnv])
        P.op(P.pool, lambda e: e.tensor_tensor(out=lnv[:, 1:2], in0=lnv[:, 0:1], in1=mhalf[:], op=ALU.pow),
             reads=[b_lnv, b_mhalf], writes=[b_lnv])
        P.op(P.pool, lambda e: e.tensor_scalar(out=lnv[:, 2:3], in0=mv[:, 0:1], scalar1=-1.0, scalar2=lnv[:, 1:2],
                                               op0=ALU.mult, op1=ALU.mult), reads=[b_mv, b_lnv], writes=[b_lnv])
        P.op(P.act, lambda e: e.activation(out=h[:], in_=r[:], func=AF.Identity, bias=lnv[:, 2:3], scale=lnv[:, 1:2]),
             reads=[br, b_lnv], writes=[bh])
        if not split:
            ln_affine(h, bh, g, b, bgb, g_on_pool)

    def ln_affine(h, bh, g, b, bgb, g_on_pool=False):
        ge_ = P.pool if g_on_pool else P.dve
        P.op(ge_, lambda e: e.tensor_tensor(out=h[:], in0=h[:], in1=g[:], op=ALU.mult), reads=[bh, bgb], writes=[bh])
        P.op(P.pool, lambda e: e.tensor_tensor(out=h[:], in0=h[:], in1=b[:], op=ALU.add), reads=[bh, bgb], writes=[bh])

    import itertools

    def drive(gA, gBs):
        gBs = list(gBs)
        aA = gA is not None
        while aA or gBs:
            if aA:
                try:
                    next(gA)
                except StopIteration:
                    aA = False
            for g in list(gBs):
                try:
                    next(g)
                except StopIteration:
                    gBs.remove(g)

    for kt in range(min(5, NT)):
        load_kv(kt)
    load_q(0)
    drive(stage1(0), [])
    load_merge_w()
    for t in range(NT + 4):
        if t + 5 < NT:
            load_kv(t + 5)
        if t + 1 < NT:
            load_q(t + 1)
        if 0 <= t - 1 < NT:
            load_x(t - 1)
        if 0 <= t - 3 < NT:
            s2_ln(t - 3)
        gens = []
        if t < NT:
            gens.append(itertools.chain(s2_pv(t), s2_tr(t)))
        if 0 <= t - 1 < NT:
            gens.append(s2_br(t - 1))
        if 0 <= t - 2 < NT:
            gens.append(s2_mix_b(t - 2))
        if 0 <= t - 4 < NT:
            gens.append(s2_ln_b(t - 4))
        drive(stage1(t + 1) if t + 1 < NT else None, gens)
    pop()
    rot["n"] = 8; rot["strict"] = False
    if stop_after == "A2":
        return nc

    push()
    Wg_r = [P.sb(f"Wg{i}", [128, 8, 512], BF16) for i in range(3)]; b_Wg = [B(f"Wg{i}") for i in range(3)]
    Wu_r = [P.sb(f"Wu{i}", [128, 8, 512], BF16) for i in range(3)]; b_Wu = [B(f"Wu{i}") for i in range(3)]
    Wd_r = [P.sb(f"Wd{i}", [128, 4, D], BF16) for i in range(4)]; b_Wd = [B(f"Wd{i}") for i in range(4)]
    def load_gu(n):
        ex, fb = divmod(n, 4); sl = n % 3
        P.dma(P.pool, lambda e: e.dma_start(out=Wg_r[sl][:], in_=wg_d[ex].rearrange("(k p) n -> p k n", p=128)[:, :, fb * 512:(fb + 1) * 512]),
              b_Wg[sl], writes=[b_Wg[sl]])
        P.dma(P.pool, lambda e: e.dma_start(out=Wu_r[sl][:], in_=wu_d[ex].rearrange("(k p) n -> p k n", p=128)[:, :, fb * 512:(fb + 1) * 512]),
              b_Wu[sl], writes=[b_Wu[sl]])

    def load_d(ex):
        for fb in range(4):
            P.dma(P.pool, lambda e: e.dma_start(out=Wd_r[fb][:], in_=wd_d[ex, fb * 512:(fb + 1) * 512, :].rearrange("(c p) n -> p c n", p=128)),
                  b_Wd[fb], writes=[b_Wd[fb]])

    load_gu(0); load_gu(1); load_d(0)

    push()
    G8 = 8; SG = S // G8
    affS = P.sb("affS", [128, SG], F32); b_affS = B("affS")
    P.dma(P.sp, lambda e: e.dma_start(out=affS[:], in_=affT_d.rearrange("e (g n) -> (e g) n", g=G8)), b_affS,
          reads=[B_affT], writes=[b_affS])
    junk = P.sb("junk", [128, SG], F32); b_junk = B("junk")
    onesS = P.sb("onesS", [128, SG], F32); b_ones = B("onesS")
    Cs = P.sb("Cs", [128, SG], F32); b_Cs = B("Cs")
    bis = P.sb("bis", [128, 8], F32); b_bis = B("bis")
    BD = P.sb("BD", [128, 128], F32); LT = P.sb("LT", [128, 128], F32); b_BD = B("BD")
    P.dma(P.sp, lambda e: e.dma_start(out=BD[:], in_=bd_d[:, :]), B("BDl"), adds=[b_BD])
    P.dma(P.sp, lambda e: e.dma_start(out=LT[:], in_=lt_d[:, :]), B("LTl"), adds=[b_BD])
    P.op(P.pool, lambda e: e.memset(onesS[:], 1.0), writes=[b_ones])
    P.op(P.dve, lambda e: e.memset(bis[:, 0:1], 0.0), writes=[b_bis])
    P.op(P.dve, lambda e: e.memset(bis[:, 1:2], 1.0), reads=[b_bis], writes=[b_bis])
    lo, hi, mid, cn, ge, dd = [bis[:, i:i + 1] for i in range(6)]
    for it in range(32):
        c = 2.0 ** -(it + 1)
        P.op(P.dve, lambda e: e.tensor_scalar(out=mid, in0=lo, scalar1=c, scalar2=None, op0=ALU.add),
             reads=[b_bis], writes=[b_bis])
        P.op(P.dve, lambda e: e.tensor_scalar(out=junk[:], in0=affS[:], scalar1=mid, scalar2=None, op0=ALU.is_ge,
                                              op1=ALU.add, accum_out=cn), reads=[b_bis, b_affS], writes=[b_bis, b_junk])
        bk, bbk = nbank()
        P.op(P.pe, lambda e: e.matmul(bk[:, 0:1], lhsT=BD[:], rhs=cn, start=True, stop=True),
             reads=[b_BD, b_bis], writes=[bbk])
        P.op(P.dve, lambda e: e.tensor_scalar(out=dd, in0=bk[:, 0:1], scalar1=float(CAP) - 0.5, scalar2=c,
                                              op0=ALU.is_ge, op1=ALU.mult), reads=[bbk, b_bis], writes=[b_bis])
        P.op(P.dve, lambda e: e.tensor_tensor(out=lo, in0=lo, in1=dd, op=ALU.add), reads=[b_bis], writes=[b_bis])
    P.op(P.dve, lambda e: e.tensor_scalar(out=junk[:], in0=affS[:], scalar1=lo, scalar2=None, op0=ALU.is_ge),
         reads=[b_bis, b_affS], writes=[b_junk])
    P.op(P.dve, lambda e: e.tensor_tensor_scan(out=Cs[:], data0=onesS[:], data1=junk[:], initial=0.0,
                                               op0=ALU.mult, op1=ALU.add), reads=[b_ones, b_junk], writes=[b_Cs])
    bk, bbk = nbank()
    P.op(P.pe, lambda e: e.matmul(bk[:, 0:1], lhsT=LT[:], rhs=Cs[:, SG - 1:SG], start=True, stop=True),
         reads=[b_BD, b_Cs], writes=[bbk])
    P.op(P.dve, lambda e: e.tensor_copy(out=bis[:, 6:7], in_=bk[:, 0:1]), reads=[bbk, b_bis], writes=[b_bis])
    P.op(P.dve, lambda e: e.tensor_scalar(out=Cs[:], in0=Cs[:], scalar1=bis[:, 6:7], scalar2=None, op0=ALU.add),
         reads=[b_bis, b_Cs], writes=[b_Cs])
    b_Cd = B("Cd")
    P.dma(P.sp, lambda e: e.dma_start(out=C_d.rearrange("e (g n) -> (e g) n", g=G8), in_=Cs[:]), b_Cs,
          reads=[b_Cs], writes=[b_Cd])
    C3 = C_d.rearrange("e (t p) -> t e p", p=128)
    Ct = P.sb("Ct", [NT, 16, 129], F32); b_Ct = B("Ct")
    At = P.sb("At", [NT, 16, 128], F32); b_At = B("At")
    CTp = P.sb("CTp", [NT, 16, 1], F32); b_CTp = B("CTp")
    P.dma(P.sp, lambda e: e.dma_start(out=Ct[:, :, 0:128], in_=C3), b_Ct, reads=[b_Cd], writes=[b_Ct])
    P.op(P.pool, lambda e: e.iota(Ct[:, :, 128:129], pattern=[[0, 16]], base=0, channel_multiplier=128,
                                  allow_small_or_imprecise_dtypes=True), reads=[b_Ct], writes=[b_Ct])
    P.dma(P.sp, lambda e: e.dma_start(out=At[:], in_=affT_d.rearrange("e (t p) -> t e p", p=128)), b_At,
          reads=[B_affT], writes=[b_At])
    P.op(P.dve, lambda e: e.memset(CTp[:], 0.0), writes=[b_CTp])
    P.dma(P.sp, lambda e: e.dma_start(out=CTp[1:NT, :, :], in_=C3[0:NT - 1, :, 127:128], allow_slow_non_contiguous=True), b_CTp,
          reads=[b_Cd], writes=[b_CTp])
    iota_c = P.sb("iota_c", [NT, CAP], F32); cg = P.sb("cg", [128, NJ], F32); iota_p = P.sb("iota_p", [128, 128], F32)
    b_iota = B("iota")
    P.op(P.pool, lambda e: e.iota(iota_c[:], pattern=[[1, CAP]], base=0, channel_multiplier=0,
                                  allow_small_or_imprecise_dtypes=True), writes=[b_iota])
    P.op(P.pool, lambda e: e.iota(cg[:], pattern=[[128, NJ]], base=0, channel_multiplier=1,
                                  allow_small_or_imprecise_dtypes=True), writes=[b_iota])
    P.op(P.pool, lambda e: e.iota(iota_p[:], pattern=[[1, 128]], base=0, channel_multiplier=0,
                                  allow_small_or_imprecise_dtypes=True), writes=[b_iota])
    a_t = P.sb("a_t", [NT, CAP], F32); oh = P.sb("oh", [NT, CAP], F32); b_at = B("a_t"); b_oh = B("oh")
    jk = [P.sb(f"jk{i}", [128, 128], F32) for i in range(2)]; b_jk = [B(f"jk{i}") for i in range(2)]
    nloc = P.sb("nloc", [128, 2], F32); b_nloc = B("nloc")
    for ex in range(16):
        P.op(P.dve, lambda e: e.tensor_scalar(out=a_t[:], in0=iota_c[:], scalar1=CTp[:, ex, :], scalar2=None,
                                              op0=ALU.is_lt), reads=[b_iota, b_CTp], writes=[b_at])
        P.op(P.dve, lambda e: e.scalar_tensor_tensor(out=oh[:], in0=iota_c[:], scalar=Ct[:, ex, 127:128], in1=a_t[:],
                                                     op0=ALU.is_lt, op1=ALU.subtract),
             reads=[b_iota, b_Ct, b_at], writes=[b_oh])
        for j in range(NJ):
            bk, bbk = nbank()
            P.op(P.pe, lambda e: e.matmul(bk[:, 0:129], lhsT=oh[:, j * 128:(j + 1) * 128], rhs=Ct[:, ex, :],
                                          start=True, stop=True), reads=[b_oh, b_Ct], writes=[bbk])
            P.op(P.pe, lambda e: e.matmul(bk[:, 256:384], lhsT=oh[:, j * 128:(j + 1) * 128], rhs=At[:, ex, :],
                                          start=True, stop=True), reads=[b_oh, b_At], writes=[bbk])
            P.op(P.dve, lambda e: e.tensor_scalar(out=jk[0][:], in0=bk[:, 0:128], scalar1=cg[:, j:j + 1], scalar2=None,
                                                  op0=ALU.is_le, op1=ALU.add, accum_out=nloc[:, 0:1]),
                 reads=[bbk, b_iota], writes=[b_jk[0], b_nloc])
            P.op(P.dve, lambda e: e.tensor_tensor(out=idx_all[:, ex, j:j + 1], in0=bk[:, 128:129], in1=nloc[:, 0:1],
                                                  op=ALU.add), reads=[bbk, b_nloc], writes=[b_idx])
            P.op(P.dve, lambda e: e.scalar_tensor_tensor(out=jk[1][:], in0=iota_p[:], scalar=nloc[:, 0:1],
                                                         in1=bk[:, 256:384], op0=ALU.is_equal, op1=ALU.mult,
                                                         accum_out=gates[:, ex, j:j + 1]),
                 reads=[bbk, b_nloc, b_iota], writes=[b_jk[1], b_gates])
    pop()
    if stop_after == "B":
        return nc

    push()
    TH = max(1, CAP // 512); TN = CAP // TH
    XT = [P.sb(f"XT{i}", [128, 8, CAP], BF16) for i in range(2)]; b_XT = [B(f"XT{i}") for i in range(2)]
    actT = P.sb("actT", [128, 16, CAP], BF16); b_actT = B("actT")
    Xg = [P.sb(f"Xg{i}", [128, D], BF16) for i in range(3)]; b_Xg = [B(f"Xg{i}") for i in range(3)]
    sg = [P.sb(f"sg{i}", [128, 512], F32) for i in range(2)]; b_sg = [B(f"sg{i}") for i in range(2)]
    ysb = [P.sb(f"ysb{i}", [128, D], F32) for i in range(3)]; b_ysb = [B(f"ysb{i}") for i in range(3)]
    Wv = [B(f"wave{i}") for i in range(16)]
    B_scat = B("scat")
    cc = {"xg": 0, "sg": 0, "y": 0}

    def gather(ex):
        xt = XT[ex % 2]; bxt = b_XT[ex % 2]
        for j in range(NJ):
            sl = cc["xg"] % 3; cc["xg"] += 1
            P.dma(P.pool, lambda e: e.indirect_dma_start(out=Xg[sl][:, :], out_offset=None, in_=h16_d[:, :],
                                                         in_offset=bass.IndirectOffsetOnAxis(ap=idx_all[:, ex, j:j + 1], axis=0)),
                  b_Xg[sl], reads=[b_idx, B_h16], writes=[b_Xg[sl]])
            bk, bbk = nbank()
            bkb = bk[:].bitcast(BF16)
            for k in range(8):
                P.op(P.pe, lambda e: e.transpose(out=bkb[:, k * 128:(k + 1) * 128], in_=Xg[sl][:, k * 128:(k + 1) * 128],
                                                 identity=identb[:]), reads=[b_Xg[sl], b_id], writes=[bbk])
            P.op(P.act, lambda e: e.copy(out=xt[:, :, j * 128:(j + 1) * 128],
                                         in_=bkb[:, 0:1024].rearrange("p (k n) -> p k n", n=128)),
                 reads=[bbk], writes=[bxt])

    gather(0)
    for ex in range(16):
        xt = XT[ex % 2]; bxt = b_XT[ex % 2]
        for fb in range(4):
            n = 4 * ex + fb
            if n + 2 < 64:
                load_gu(n + 2)
            sl = n % 3
            for f4 in range(4):
                fc = 4 * fb + f4
                for th in range(TH):
                    bkG, bbG = nbank()
                    for k in range(8):
                        P.op(P.pe, lambda e: e.matmul(bkG[:, 0:TN], lhsT=Wg_r[sl][:, k, f4 * 128:(f4 + 1) * 128],
                                                      rhs=xt[:, k, th * TN:(th + 1) * TN], start=(k == 0), stop=(k == 7)),
                             reads=[b_Wg[sl], bxt], writes=[bbG])
                    bkU, bbU = nbank()
                    for k in range(8):
                        P.op(P.pe, lambda e: e.matmul(bkU[:, 0:TN], lhsT=Wu_r[sl][:, k, f4 * 128:(f4 + 1) * 128],
                                                      rhs=xt[:, k, th * TN:(th + 1) * TN], start=(k == 0), stop=(k == 7)),
                             reads=[b_Wu[sl], bxt], writes=[bbU])
                    si = cc["sg"] % 2; cc["sg"] += 1
                    P.op(P.act, lambda e: e.activation(out=sg[si][:, 0:TN], in_=bkG[:, 0:TN], func=AF.Silu),
                         reads=[bbG], writes=[b_sg[si]])
                    P.op(P.dve, lambda e: e.tensor_tensor(out=actT[:, fc, th * TN:(th + 1) * TN], in0=bkU[:, 0:TN],
                                                          in1=sg[si][:, 0:TN], op=ALU.mult),
                         reads=[bbU, b_sg[si]], writes=[b_actT])
        if ex + 1 < 16:
            gather(ex + 1)
        for j in range(NJ):
            yi = cc["y"] % 3; cc["y"] += 1
            for chh in range(2):
                bk, bbk = nbank()
                for fc in range(16):
                    P.op(P.pe, lambda e: e.matmul(bk[:, 0:512], lhsT=actT[:, fc, j * 128:(j + 1) * 128],
                                                  rhs=Wd_r[fc // 4][:, fc % 4, chh * 512:(chh + 1) * 512],
                                                  start=(fc == 0), stop=(fc == 15)),
                         reads=[b_actT, b_Wd[fc // 4]], writes=[bbk])
                P.op(P.act, lambda e: e.activation(out=ysb[yi][:, chh * 512:(chh + 1) * 512], in_=bk[:, 0:512],
                                                   func=AF.Identity, scale=gates[:, ex, j:j + 1]),
                     reads=[bbk, b_gates], writes=[b_ysb[yi]])
            prev = [Wv[ex - 1]] if ex > 0 else [B_acc]
            P.dma(P.pool, lambda e: e.indirect_dma_start(out=acc_d[:, :],
                                                         out_offset=bass.IndirectOffsetOnAxis(ap=idx_all[:, ex, j:j + 1], axis=0),
                                                         in_=ysb[yi][:, :], in_offset=None, compute_op=ALU.add),
                  b_ysb[yi], reads=[b_ysb[yi], b_idx] + prev, adds=[Wv[ex], B_scat])
        if ex + 1 < 16:
            load_d(ex + 1)
    pop()
    pop()
    if stop_after == "C":
        return nc

    push()
    ln2g = P.sb("ln2g", [128, D], F32); ln2b = P.sb("ln2b", [128, D], F32); b_ln2 = B("ln2")
    P.dma(P.sp, lambda e: e.dma_start(out=ln2g[:], in_=ln2g_d[:, :]), B("ln2g"), adds=[b_ln2])
    P.dma(P.sp, lambda e: e.dma_start(out=ln2b[:], in_=ln2b_d[:, :]), B("ln2b"), adds=[b_ln2])
    ND = 12
    at = [P.sb(f"at{i}", [128, D], F32) for i in range(ND)]; b_at2 = [B(f"at{i}") for i in range(ND)]
    ot = [P.sb(f"ot{i}", [128, D], F32) for i in range(ND)]; b_ot = [B(f"ot{i}") for i in range(ND)]
    def d_load(t):
        P.dma(P.sp, lambda e: e.dma_start(out=at[t % ND][:], in_=acc_d[t * 128:(t + 1) * 128, :]), b_at2[t % ND],
              reads=[B_acc, B_scat], writes=[b_at2[t % ND]])

    for t in range(min(ND - 1, NT)):
        d_load(t)
    def d_norm(t):
        layer_norm(at[t % ND], b_at2[t % ND], ot[t % ND], b_ot[t % ND], ln2g, ln2b, b_ln2, split=True)

    d_norm(0)
    for t in range(NT):
        o = ot[t % ND]; bo = b_ot[t % ND]
        if t + ND - 1 < NT:
            d_load(t + ND - 1)
        if t + 1 < NT:
            d_norm(t + 1)
        ln_affine(o, bo, ln2g, ln2b, b_ln2)
        P.dma(P.sp, lambda e: e.dma_start(out=out_d[t * 128:(t + 1) * 128, :], in_=o[:]), bo, reads=[bo])
    pop()
    return nc


def _perm():
    qa = np.arange(0, 512)
    qb = np.concatenate([np.concatenate([1536 + np.arange(64 * i, 64 * i + 64),
                                         1536 + np.arange(64 * (i + 4), 64 * (i + 4) + 64)]) for i in range(4)])
    ga = np.arange(2304, 3328); gb = np.arange(3328, 4352)
    ka = np.arange(512, 1024); kb = np.arange(2048, 2176)
    va = np.arange(1024, 1536); vb = np.arange(2176, 2304)
    return np.concatenate([qa, qb, ga, gb, ka, kb, va, vb])


def _na_bias(rpb, t, kts, rows):
    q = np.arange(128); r = 2 * t + q // 64; c = q % 64
    r0 = np.clip(r - 4, 0, rows - 8); cs = np.clip(c - 8, 0, 48)
    k = np.arange(128)
    out = np.full((128, 8, len(kts), 128), NEG, np.float32)
    for i, kt in enumerate(kts):
        rk = 2 * kt + k // 64; ck = k % 64
        valid = ((rk[:, None] >= r0[None, :]) & (rk[:, None] < r0[None, :] + 8)
                 & (ck[:, None] >= cs[None, :]) & (ck[:, None] < cs[None, :] + 16))
        dr = np.clip(rk[:, None] - r[None, :] + 7, 0, 14)
        dc = np.clip(ck[:, None] - c[None, :] + 15, 0, 30)
        vals = rpb[:, dr, dc]
        out[:, :, i, :] = np.where(valid[:, None, :], vals.transpose(1, 0, 2), np.float32(NEG))
    return np.ascontiguousarray(out[:, HORD]).reshape(128, 8 * len(kts) * 128)


def _kts(t, NT):
    if t < 2:
        return [0, 1, 2, 3]
    if t >= NT - 2:
        return [NT - 4, NT - 3, NT - 2, NT - 1]
    return [t - 2, t - 1, t, t + 1, t + 2]


def _wg_bias():
    k = np.arange(128)[:, None]; q = np.arange(128)[None, :]
    out = np.zeros((128, 3, 2, 4, 128), np.float32)
    for di, dt in enumerate((-1, 0, 1)):
        dist = np.abs(q - k - 128 * dt).astype(np.float32)
        for j in range(2):
            for g in range(4):
                slope = np.float32(2.0 ** (-(4 * j + g + 1)))
                out[:, di, j, g, :] = np.where(dist <= 128, -slope * dist, np.float32(NEG))
    return out.reshape(128, 24 * 128)


def _prep_shared(inp, S):
    NT = S // 128; rows = S // 64
    f = lambda a: np.ascontiguousarray(np.asarray(a), dtype=np.float32)
    perm = _perm()
    w_in = f(inp["w_in"])[0]; b_in = f(inp["b_in"])[0]
    b_p = b_in[perm]
    rpb = f(inp["rpb"])[0]
    rep = lambda v: np.ascontiguousarray(np.broadcast_to(v[None, :], (128, v.shape[0])))
    edge_tiles = [0, 1, NT - 2, NT - 1]
    sh = {
        "w_in_p": np.ascontiguousarray(w_in[:, perm]),
        "bfm": np.ascontiguousarray(b_p[:FMW].reshape(NQ, 128).T),
        "bv": rep(b_p[FMW:]),
        "nab_int": _na_bias(rpb, 2, _kts(2, NT), rows),
        "nab_edge": np.stack([_na_bias(rpb, t, _kts(t, NT), rows) for t in edge_tiles]),
        "wgb": _wg_bias(),
        "sinkb": rep(f(inp["sink"])[0]),
        "w_a": f(inp["w_branch_a"])[0], "w_b": f(inp["w_branch_b"])[0], "w_o": f(inp["w_out"])[0],
        "w_r": f(inp["w_router"])[0],
        "ln1g": rep(f(inp["ln1_g"])[0]), "ln1b": rep(f(inp["ln1_b"])[0]),
        "ln2g": rep(f(inp["ln2_g"])[0]), "ln2b": rep(f(inp["ln2_b"])[0]),
        "bd_c": (np.arange(128)[:, None] // 8 == np.arange(128)[None, :] // 8).astype(np.float32),
        "lt_c": ((np.arange(128)[:, None] // 8 == np.arange(128)[None, :] // 8)
                 & (np.arange(128)[:, None] < np.arange(128)[None, :])).astype(np.float32),
        "w_gate": f(inp["w_gate"])[0], "w_up": f(inp["w_up"])[0], "w_down": f(inp["w_down"])[0],
    }
    return sh


def _prep_core(xb):
    xb = np.ascontiguousarray(np.asarray(xb), dtype=np.float32)
    return {"xT": np.ascontiguousarray(xb.T), "x": xb}


def kernel(**inputs):
    x = np.asarray(inputs["x"])
    Bn, S, _ = x.shape
    nc = build(S)
    sh = _prep_shared(inputs, S)
    in_maps = []
    for b in range(Bn):
        m = dict(sh)
        m.update(_prep_core(x[b]))
        in_maps.append(m)
    res = run_bass_kernel_spmd(nc, in_maps, core_ids=list(range(Bn)))
    return np.stack([np.asarray(r["out"]) for r in res.results], axis=0).astype(np.float32)
```
